# Optimizing a Trainium2 kernel written in Bass

```python
import jax, jax.numpy as jnp
from jax import lax
import numpy as np

D_MODEL = 2048
BATCH = 2
SEQ = 4096
DEPTH = 4

GRID_W = 64
CTX_LEN = 256
CHUNK = 128
SGU_GROUPS = 8
SGU_WIDTH = 2048
RET_HEADS = 8
RET_QK_DIM = 256
RET_V_DIM = 512
RET_QK = RET_HEADS * RET_QK_DIM
RET_V = RET_HEADS * RET_V_DIM
ROPE_BASE = 10000.0
D_FF = 5632
N_EXPERTS = 8
TOP_K = 2
D_FF_EXPERT = 4096
N_DENSE = (DEPTH + 1) // 2
N_MOE = DEPTH // 2
N_MOD = 6
NORM_EPS = 1e-6

Q0 = 0
K0 = Q0 + RET_QK
V0 = K0 + RET_QK
G0 = V0 + RET_V
UA0 = G0 + RET_V
VA0 = UA0 + SGU_WIDTH
GA0 = VA0 + SGU_WIDTH
GR0 = GA0 + D_MODEL
IN_COLS = GR0 + D_MODEL

kernel_name = "hybrid_sgu_retention_moe_dit"


def rms_norm(x, g):
    xf = x.astype(jnp.float32)
    y = xf * lax.rsqrt(jnp.mean(xf * xf, axis=-1, keepdims=True) + NORM_EPS)
    return (y * g.astype(jnp.float32)).astype(x.dtype)


def layer_norm(x):
    xf = x.astype(jnp.float32)
    mu = jnp.mean(xf, axis=-1, keepdims=True)
    var = jnp.mean(jnp.square(xf - mu), axis=-1, keepdims=True)
    return ((xf - mu) * lax.rsqrt(var + NORM_EPS)).astype(x.dtype)


def modulate(x, g, shift, scale):
    return rms_norm(x, g) * (1 + scale) + shift


def rope_1d(x, pos):
    half = x.shape[-1] // 2
    freqs = ROPE_BASE ** (-jnp.arange(half, dtype=jnp.float32) / half)
    ang = pos.astype(jnp.float32)[:, None] * freqs[None, :]
    cos = jnp.cos(ang).astype(x.dtype)
    sin = jnp.sin(ang).astype(x.dtype)
    x1, x2 = x[..., :half], x[..., half:]
    return jnp.concatenate([x1 * cos - x2 * sin, x1 * sin + x2 * cos], axis=-1)


def axial_rope(x, rows, cols):
    half = x.shape[-1] // 2
    return jnp.concatenate([rope_1d(x[..., :half], rows), rope_1d(x[..., half:], cols)], axis=-1)


def spatial_gating(u, v, ln_g, ln_b, w_s, b_s):
    bsz, length, width = v.shape
    vn = layer_norm(v) * ln_g + ln_b
    vh = vn.reshape(bsz, length // CHUNK, CHUNK, SGU_GROUPS, width // SGU_GROUPS)
    s = jnp.einsum('gpq,bnqgc->bnpgc', w_s, vh) + b_s.T[:, :, None]
    return u * s.reshape(bsz, length, width)


def retention_scan(q, k, v, log_g, s0):
    bsz, heads, length, _ = q.shape
    dv = v.shape[-1]
    n_chunks = length // CHUNK
    idx = jnp.arange(CHUNK, dtype=jnp.float32)
    diff = idx[:, None] - idx[None, :]
    intra = jnp.where(diff >= 0, jnp.exp(jnp.maximum(diff, 0.0)[None] * log_g[:, None, None]), 0.0)
    q_decay = jnp.exp((idx + 1.0)[None, :] * log_g[:, None])
    k_decay = jnp.exp((CHUNK - 1.0 - idx)[None, :] * log_g[:, None])
    chunk_decay = jnp.exp(CHUNK * log_g)

    def to_chunks(t):
        return jnp.moveaxis(t.reshape(bsz, heads, n_chunks, CHUNK, t.shape[-1]), 2, 0)

    def step(s, qkv):
        qc, kc, vc = qkv
        scores = jnp.einsum('bhqd,bhkd->bhqk', qc, kc) * intra
        o = (jnp.einsum('bhqk,bhkv->bhqv', scores, vc)
             + jnp.einsum('bhqd,bhdv->bhqv', qc, s) * q_decay[..., None])
        s = s * chunk_decay[:, None, None] + jnp.einsum('bhkd,bhkv->bhdv', kc * k_decay[..., None], vc)
        return s, o

    s_fin, o = lax.scan(step, s0, (to_chunks(q), to_chunks(k), to_chunks(v)))
    o = jnp.moveaxis(o, 0, 2).reshape(bsz, heads, length, dv)
    return o, s_fin


def retention_state(k, v, log_g):
    length = k.shape[2]
    w = jnp.exp((length - 1.0 - jnp.arange(length, dtype=jnp.float32))[None, :] * log_g[:, None])
    return jnp.einsum('bhld,bhlv->bhdv', k * w[..., None], v)


def bidir_retention(q_c, k_c, v_c, q_l, k_l, v_l, log_decay, ctx_out):
    flip = lambda t: jnp.flip(t, axis=2)
    g_f, g_b = log_decay[0].astype(jnp.float32), log_decay[1].astype(jnp.float32)
    zero = jnp.zeros(k_c.shape[:2] + (k_c.shape[-1], v_c.shape[-1]), jnp.float32)
    if ctx_out:
        o_cf, s_f = retention_scan(q_c, k_c, v_c, g_f, zero)
        o_cb_r, s_b = retention_scan(flip(q_c), flip(k_c), flip(v_c), g_b, zero)
        o_ctx = o_cf + flip(o_cb_r)
    else:
        s_f = retention_state(k_c, v_c, g_f)
        s_b = retention_state(flip(k_c), flip(v_c), g_b)
        o_ctx = None
    o_lf, _ = retention_scan(q_l, k_l, v_l, g_f, s_f)
    o_lb_r, _ = retention_scan(flip(q_l), flip(k_l), flip(v_l), g_b, s_b)
    return o_lf + flip(o_lb_r), o_ctx


def to_heads(t, head_dim):
    bsz, length, _ = t.shape
    return jnp.transpose(t.reshape(bsz, length, RET_HEADS, head_dim), (0, 2, 1, 3)).astype(jnp.float32)


def hybrid_mixer(h_lat, h_ctx, rows, cols, w_in, sgu_ln_g, sgu_ln_b, sgu_w, sgu_b, log_decay,
                 w_proj_a, w_proj_r, w_out, ctx_out):
    k_scale = RET_QK_DIM ** -0.5
    p_lat = h_lat @ w_in
    q_l = axial_rope(to_heads(p_lat[..., Q0:K0], RET_QK_DIM), rows, cols)
    k_l = axial_rope(to_heads(p_lat[..., K0:V0], RET_QK_DIM), rows, cols) * k_scale
    v_l = to_heads(p_lat[..., V0:G0], RET_V_DIM)
    if ctx_out:
        p_ctx = h_ctx @ w_in
        q_c = to_heads(p_ctx[..., Q0:K0], RET_QK_DIM)
        kv_c = p_ctx[..., K0:G0]
    else:
        p_ctx = None
        q_c = None
        kv_c = h_ctx @ w_in[:, K0:G0]
    k_c = to_heads(kv_c[..., :RET_QK], RET_QK_DIM) * k_scale
    v_c = to_heads(kv_c[..., RET_QK:], RET_V_DIM)
    o_lat, o_ctx = bidir_retention(q_c, k_c, v_c, q_l, k_l, v_l, log_decay, ctx_out)

    def merge(p, o_ret):
        bsz, length, _ = p.shape
        ret = jnp.transpose(layer_norm(o_ret), (0, 2, 1, 3)).reshape(bsz, length, RET_V).astype(p.dtype)
        ret = ret * jax.nn.silu(p[..., G0:UA0])
        u = jax.nn.gelu(p[..., UA0:VA0])
        v = jax.nn.gelu(p[..., VA0:GA0])
        sgu = spatial_gating(u, v, sgu_ln_g, sgu_ln_b, sgu_w, sgu_b)
        y = (jax.nn.sigmoid(p[..., GA0:GR0]) * (sgu @ w_proj_a)
             + jax.nn.sigmoid(p[..., GR0:]) * (ret @ w_proj_r))
        return y @ w_out

    out_lat = merge(p_lat, o_lat)
    out_ctx = merge(p_ctx, o_ctx) if ctx_out else None
    return out_lat, out_ctx


def swiglu(h, w1, w3, w2):
    return (jax.nn.silu(h @ w1) * (h @ w3)) @ w2


def moe_swiglu(h, router_w, router_b, w1, w3, w2):
    logits = (h @ router_w).astype(jnp.float32) + router_b.astype(jnp.float32)
    top_v, top_i = lax.top_k(logits, TOP_K)
    probs = jax.nn.softmax(top_v, axis=-1)
    gates = jnp.sum(jax.nn.one_hot(top_i, N_EXPERTS, dtype=jnp.float32) * probs[..., None], axis=-2)
    gates = gates.astype(h.dtype)
    y = jnp.zeros_like(h)
    for e in range(N_EXPERTS):
        y = y + gates[..., e:e + 1] * swiglu(h, w1[e], w3[e], w2[e])
    return y


def setup_inputs(seed: int = 0) -> dict:
    key = jax.random.key(seed)
    ks = jax.random.split(key, 26)
    f32 = jnp.float32

    def nrm(k, shape, scale):
        return jax.random.normal(k, shape, f32) * scale

    base = jnp.log1p(-(2.0 ** (-5.0 - jnp.arange(RET_HEADS, dtype=f32))))
    ret_log_decay = base * jnp.exp(0.1 * jax.random.normal(ks[13], (DEPTH, 2, RET_HEADS), f32))
    return {
        'x': nrm(ks[0], (BATCH, SEQ, D_MODEL), 1.0),
        'c': nrm(ks[1], (BATCH, D_MODEL), 1.0),
        'ctx': nrm(ks[2], (BATCH, CTX_LEN, D_MODEL), 1.0),
        'c_ctx': nrm(ks[3], (D_MODEL,), 1.0),
        'w_mod': nrm(ks[4], (DEPTH, D_MODEL, N_MOD * D_MODEL), 0.5 * D_MODEL ** -0.5),
        'b_mod': nrm(ks[5], (DEPTH, N_MOD * D_MODEL), 0.02),
        'norm1_g': 1.0 + nrm(ks[6], (DEPTH, D_MODEL), 0.02),
        'norm2_g': 1.0 + nrm(ks[7], (DEPTH, D_MODEL), 0.02),
        'w_in': nrm(ks[8], (DEPTH, D_MODEL, IN_COLS), D_MODEL ** -0.5),
        'sgu_ln_g': 1.0 + nrm(ks[9], (DEPTH, SGU_WIDTH), 0.02),
        'sgu_ln_b': nrm(ks[10], (DEPTH, SGU_WIDTH), 0.02),
        'sgu_w': nrm(ks[11], (DEPTH, SGU_GROUPS, CHUNK, CHUNK), CHUNK ** -0.5),
        'sgu_b': 1.0 + nrm(ks[12], (DEPTH, SGU_GROUPS, CHUNK), 0.02),
        'ret_log_decay': ret_log_decay,
        'w_proj_a': nrm(ks[14], (DEPTH, SGU_WIDTH, D_MODEL), SGU_WIDTH ** -0.5),
        'w_proj_r': nrm(ks[15], (DEPTH, RET_V, D_MODEL), RET_V ** -0.5),
        'w_out': nrm(ks[16], (DEPTH, D_MODEL, D_MODEL), D_MODEL ** -0.5),
        'ffn_w1': nrm(ks[17], (N_DENSE, D_MODEL, D_FF), D_MODEL ** -0.5),
        'ffn_w3': nrm(ks[18], (N_DENSE, D_MODEL, D_FF), D_MODEL ** -0.5),
        'ffn_w2': nrm(ks[19], (N_DENSE, D_FF, D_MODEL), D_FF ** -0.5),
        'router_w': nrm(ks[20], (N_MOE, D_MODEL, N_EXPERTS), D_MODEL ** -0.5),
        'router_b': nrm(ks[21], (N_MOE, N_EXPERTS), 0.01),
        'moe_w1': nrm(ks[22], (N_MOE, N_EXPERTS, D_MODEL, D_FF_EXPERT), D_MODEL ** -0.5),
        'moe_w3': nrm(ks[23], (N_MOE, N_EXPERTS, D_MODEL, D_FF_EXPERT), D_MODEL ** -0.5),
        'moe_w2': nrm(ks[24], (N_MOE, N_EXPERTS, D_FF_EXPERT, D_MODEL), D_FF_EXPERT ** -0.5),
        'final_norm_g': 1.0 + nrm(ks[25], (D_MODEL,), 0.02),
    }


def reference(x, c, ctx, c_ctx, w_mod, b_mod, norm1_g, norm2_g, w_in, sgu_ln_g, sgu_ln_b, sgu_w, sgu_b,
              ret_log_decay, w_proj_a, w_proj_r, w_out, ffn_w1, ffn_w3, ffn_w2, router_w, router_b,
              moe_w1, moe_w3, moe_w2, final_norm_g):
    n_lat = x.shape[1]
    t = jnp.arange(n_lat)
    rows = t // GRID_W
    cols = t % GRID_W
    sc = jax.nn.silu(c)
    sc_ctx = jax.nn.silu(c_ctx)
    for layer in range(DEPTH):
        last = layer == DEPTH - 1
        m = [t_[:, None, :] for t_ in jnp.split(sc @ w_mod[layer] + b_mod[layer], N_MOD, axis=-1)]
        mc = jnp.split(sc_ctx @ w_mod[layer] + b_mod[layer], N_MOD, axis=-1)

        h_lat = modulate(x, norm1_g[layer], m[0], m[1])
        h_ctx = modulate(ctx, norm1_g[layer], mc[0], mc[1])
        out_lat, out_ctx = hybrid_mixer(h_lat, h_ctx, rows, cols, w_in[layer], sgu_ln_g[layer],
                                        sgu_ln_b[layer], sgu_w[layer], sgu_b[layer], ret_log_decay[layer],
                                        w_proj_a[layer], w_proj_r[layer], w_out[layer], not last)
        x = x + m[2] * out_lat
        h2 = modulate(x, norm2_g[layer], m[3], m[4])
        if not last:
            ctx = ctx + mc[2] * out_ctx
            h2 = jnp.concatenate([modulate(ctx, norm2_g[layer], mc[3], mc[4]), h2], axis=1)

        if layer % 2 == 0:
            i = layer // 2
            f = swiglu(h2, ffn_w1[i], ffn_w3[i], ffn_w2[i])
        else:
            i = layer // 2
            f = moe_swiglu(h2, router_w[i], router_b[i], moe_w1[i], moe_w3[i], moe_w2[i])

        if not last:
            n_ctx = ctx.shape[1]
            ctx = ctx + mc[5] * f[:, :n_ctx]
            f = f[:, n_ctx:]
        x = x + m[5] * f
    return rms_norm(x, final_norm_g)
```

```python
import math
from contextlib import ExitStack

import numpy as np
import concourse.bass as bass
import concourse.mybir as mybir
from concourse.bass_utils import run_bass_kernel_spmd

F32 = mybir.dt.float32
BF16 = mybir.dt.bfloat16
AF = mybir.ActivationFunctionType
ALU = mybir.AluOpType
AX = mybir.AxisListType

D = 2048
NCTX = 256
GRID_W = 64
HEADS = 8
DK = 256
DV = 512
IN_COLS = 20480
D_FF = 5632
NE = 8
D_FFE = 4096
EPS = 1e-6
Q0, K0, V0, G0, UA0, VA0, GA0, GR0 = 0, 2048, 4096, 8192, 12288, 14336, 16384, 18432


class Sched:
    def __init__(self, nc, es):
        self.nc = nc
        self.es = es
        self.eng = {"pe": nc.tensor, "act": nc.scalar, "dve": nc.vector, "pool": nc.gpsimd, "sp": nc.sync}
        self.sem = {k: es.enter_context(nc.semaphore("s_" + k)) for k in ["pe", "act", "dve", "pool", "dma"]}
        self.cnt = {k: 0 for k in self.sem}
        self.prev = []
        self.extra = []
        self.cur = []
        self.rec = None

    def token(self, name):
        return {"sem": self.es.enter_context(self.nc.semaphore(name)), "cnt": 0}

    def _q(self):
        return self.rec[-1] if self.rec is not None else self.cur

    def op(self, e, fn):
        self._q().append((e, fn, False))

    def dma(self, e, out, in_, **kw):
        self._q().append((e, lambda eng: eng.dma_start(out=out, in_=in_, **kw), True))

    def dma_async(self, e, out, in_, tok, **kw):
        self._q().append((e, lambda eng: eng.dma_start(out=out, in_=in_, **kw), tok))

    def require(self, tok):
        self.extra.append(tok)

    def require_val(self, tok, val):
        self.extra.append({"sem": tok["sem"], "cnt": val})

    def record(self, fn):
        self.rec = [[]]
        fn()
        ph = [p for p in self.rec if p]
        self.rec = None
        return ph

    def step(self):
        if self.rec is not None:
            if self.rec[-1]:
                self.rec.append([])
            return
        if not self.cur:
            return
        by = {}
        for e, fn, d in self.cur:
            by.setdefault(e, []).append((fn, d))
        used = set()
        waits = list(self.prev) + [(t["sem"], t["cnt"]) for t in self.extra]
        for e, lst in by.items():
            eng = self.eng[e]
            for sm, v in waits:
                eng.wait_ge(sm, v)
            last_c = max([i for i, (_, d) in enumerate(lst) if d is False], default=-1)
            for i, (fn, d) in enumerate(lst):
                ins = fn(eng)
                if d is True:
                    ins.then_inc(self.sem["dma"], 16)
                    self.cnt["dma"] += 16
                    used.add("dma")
                elif d is not False:
                    ins.then_inc(d["sem"], 16)
                    d["cnt"] += 16
                elif e == "pool" or i == last_c:
                    ins.then_inc(self.sem[e], 1)
                    self.cnt[e] += 1
                    used.add(e)
        self.prev = [(self.sem[k], self.cnt[k]) for k in used]
        self.extra = []
        self.cur = []


def build(n_lat, layer_kinds, final_norm=True):
    T = NCTX + n_lat
    NT = T // 128
    L = len(layer_kinds)
    n_dense = sum(1 for k in layer_kinds if k == "dense")
    n_moe = L - n_dense
    nc = bass.Bass("TRN2", target_bir_lowering=False)

    def din(name, shape):
        return nc.dram_tensor(name, list(shape), F32, kind="ExternalInput").ap()

    xin = din("xin", [T, D])
    cvec = din("cvec", [2, D])
    w_mod = din("w_mod", [L, D, 6 * D])
    b_mod = din("b_mod", [L, 6 * D])
    norm1_g = din("norm1_g", [L, D])
    norm2_g = din("norm2_g", [L, D])
    w_in = din("w_in", [L, D, IN_COLS])
    sgu_ln_g = din("sgu_ln_g", [L, D])
    sgu_ln_b = din("sgu_ln_b", [L, D])
    sgu_w = din("sgu_w", [L, 8, 128, 128])
    sgu_b = din("sgu_b", [L, 8, 128])
    ret_ld = din("ret_log_decay", [L, 16])
    w_proj_a = din("w_proj_a", [L, D, D])
    w_proj_r = din("w_proj_r", [L, 2 * D, D])
    w_out = din("w_out", [L, D, D])
    ffn_w1 = din("ffn_w1", [max(n_dense, 1), D, D_FF])
    ffn_w3 = din("ffn_w3", [max(n_dense, 1), D, D_FF])
    ffn_w2 = din("ffn_w2", [max(n_dense, 1), D_FF, D])
    router_w = din("router_w", [max(n_moe, 1), D, NE])
    router_b = din("router_b", [max(n_moe, 1), NE])
    moe_w1 = din("moe_w1", [max(n_moe, 1), NE, D, D_FFE])
    moe_w3 = din("moe_w3", [max(n_moe, 1), NE, D, D_FFE])
    moe_w2 = din("moe_w2", [max(n_moe, 1), NE, D_FFE, D])
    final_g = din("final_norm_g", [1, D])
    consts = din("consts", [128, 5 * 128 + 8])
    rope_t = din("rope_t", [max(n_lat // 128, 1), 128, 256])
    y_out = nc.dram_tensor("y_out", [n_lat, D], F32, kind="ExternalOutput").ap()
    x_out = nc.dram_tensor("x_out", [T, D], F32, kind="ExternalOutput").ap()

    def dscr(name, shape, dt):
        return nc.dram_tensor(name, list(shape), dt).ap()

    XR = dscr("XR", [T, D], F32)
    MB = dscr("MB", [2, 128, 6 * D], F32)
    HT = dscr("HT", [128, 16, T], BF16)
    QK = dscr("QK", [T, 4096], F32)
    VV = dscr("VV", [T, 4096], BF16)
    SG = dscr("SG", [T, 4096], BF16)
    UU = dscr("UU", [T, D], BF16)
    VA = dscr("VA", [T, D], F32)
    SGA = dscr("SGA", [128, 16, T], BF16)
    SGR = dscr("SGR", [128, 16, T], BF16)
    QKT = dscr("QKT", [128, 32, T], BF16)
    KFB = dscr("KFB", [T, 2, D], BF16)
    ST = dscr("ST", [NT, 2, 128, 2, 512], BF16)
    RETT = dscr("RETT", [128, 32, T], BF16)
    SGT = dscr("SGT", [128, 16, T], BF16)
    YT = dscr("YT", [128, 16, T], F32)
    YTB = dscr("YTB", [128, 16, T], BF16)
    H1T = dscr("H1T", [128, 44, T], BF16)
    HIDT = dscr("HIDT", [128, 44, T], BF16)
    GT = dscr("GT", [T, NE], F32)

    groups = [(0, NCTX)] + [(NCTX + i * 512, min(512, T - NCTX - i * 512)) for i in range((n_lat + 511) // 512)]

    with ExitStack() as es:
        S = Sched(nc, es)

        uniq = [0]

        def sb(st, name, shape, dt):
            uniq[0] += 1
            return st.enter_context(nc.sbuf_tensor("%s_%d" % (name, uniq[0]), list(shape), dt))

        def pb(st, name, shape, dt):
            uniq[0] += 1
            return st.enter_context(nc.psum_tensor("%s_%d" % (name, uniq[0]), list(shape), dt))

        cst = sb(es, "cst", [128, 5 * 128 + 8], F32)
        identb = sb(es, "identb", [128, 128], BF16)
        S.dma("sp", cst[:], consts[:, :])
        S.dma("pool", identb[:], consts[:, 0:128])
        S.dma("sp", XR.rearrange("(p a) d -> p (a d)", p=128), xin.rearrange("(p a) d -> p (a d)", p=128))
        S.step()
        ident32 = cst[:, 0:128]
        dpos = cst[:, 128:256]
        dneg = cst[:, 256:384]
        umask = cst[:, 384:512]
        lmask = cst[:, 512:640]
        col_i = cst[:, 640:641]
        col_127mi = cst[:, 641:642]
        col_ip1 = cst[:, 642:643]
        col_128mi = cst[:, 643:644]

        tok_at = [S.token("t_at%d" % i) for i in range(2)]
        tok_wb = [S.token("t_wb%d" % i) for i in range(4)]
        tok_pre = [S.token("t_pre%d" % i) for i in range(2)]
        tok_st = S.token("t_st")
        flush_t = sb(es, "flush_t", [128, 8], F32)

        def load_w_block(wb, wsrc, KC, n0, ncols, tok=None):
            for k0 in range(0, KC, 8):
                k1 = min(KC, k0 + 8)
                src = wsrc[k0 * 128:k1 * 128, n0:n0 + ncols].rearrange("(kc p) n -> p kc n", p=128)
                if tok is None:
                    S.dma("pool", wb[:, k0:k1, 0:ncols], src)
                else:
                    S.dma_async("pool", wb[:, k0:k1, 0:ncols], src, tok)

        def gemm(AT, KC, wsrc, N, form, epi_pre, epi, tag, grp=None):
            grp = grp or groups
            atw = max(n for _, n in grp)
            NB = N // 512
            PW = 2 if KC <= 16 else 1
            NWS = 2 * PW
            with ExitStack() as st:
                wb = [sb(st, "wb_" + tag, [128, KC, 512], BF16) for _ in range(NWS)]
                at = [sb(st, "at_" + tag, [128, KC, atw], BF16) for _ in range(2)]
                ps = [[pb(st, "ps%d_%s" % (i, tag), [128, 512], F32) for i in range(4)] for _ in range(2)]
                items = []
                w_issue = {}
                a_idx = -1
                prev_start = None
                for nb0 in range(0, NB, PW):
                    blks = list(range(nb0, min(NB, nb0 + PW)))
                    start = len(items)
                    if prev_start is not None:
                        for nb in blks:
                            w_issue.setdefault(prev_start + 1 + (nb - nb0), []).append(nb)
                    prev_start = start
                    for gi in range(len(grp)):
                        a_idx += 1
                        for h_, nb in enumerate(blks):
                            items.append((nb, gi, h_ == 0, a_idx))
                w_first = {}
                for i_, it in enumerate(items):
                    w_first.setdefault(it[0], i_)

                def issue_loads(i):
                    nb, gi, first, ai = items[i]
                    if first:
                        tok0, ntok = grp[gi]
                        s_ = ai % 2
                        for k0 in range(0, KC, 8):
                            k1 = min(KC, k0 + 8)
                            S.dma_async("sp", at[s_][:, k0:k1, 0:ntok], AT[:, k0:k1, tok0:tok0 + ntok], tok_at[s_])
                    for nbn in w_issue.get(i, []):
                        load_w_block(wb[nbn % NWS], wsrc, KC, nbn * 512, 512, tok_wb[nbn % NWS])

                for nb in range(min(PW, NB)):
                    load_w_block(wb[nb % NWS], wsrc, KC, nb * 512, 512, tok_wb[nb % NWS])
                issue_loads(0)
                S.step()
                hist = []
                for i in range(len(items) + 1):
                    hist.append(tok_st["cnt"])
                    if i >= 1:
                        S.require_val(tok_st, hist[i - 1])
                    phases = []
                    if i >= 1:
                        nb_, gi_ = items[i - 1][0], items[i - 1][1]
                        phases = S.record(lambda: epi(ps[(i - 1) % 2], nb_, gi_, (i - 1) % 2))
                        if epi_pre is not None:
                            S.require(tok_pre[(i - 1) % 2])
                    mm = []
                    if i < len(items):
                        nb, gi, first, ai = items[i]
                        tok0, ntok = grp[gi]
                        a_, w_, p_ = at[ai % 2], wb[nb % NWS], ps[i % 2]
                        if form == "tm":
                            for j in range(ntok // 128):
                                for kc in range(KC):
                                    mm.append(("pe", lambda e, j=j, kc=kc, a_=a_, w_=w_, p_=p_: e.matmul(
                                        p_[j][:, :], lhsT=a_[:, kc, j * 128:(j + 1) * 128], rhs=w_[:, kc, :],
                                        start=(kc == 0), stop=(kc == KC - 1)), False))
                        else:
                            for cb in range(4):
                                for kc in range(KC):
                                    mm.append(("pe", lambda e, cb=cb, kc=kc, a_=a_, w_=w_, p_=p_, ntok=ntok: e.matmul(
                                        p_[cb][:, 0:ntok], lhsT=w_[:, kc, cb * 128:(cb + 1) * 128],
                                        rhs=a_[:, kc, 0:ntok], start=(kc == 0), stop=(kc == KC - 1)), False))
                        if first:
                            S.require(tok_at[ai % 2])
                        if w_first[nb] == i:
                            S.require(tok_wb[nb % NWS])
                    nparts = max(len(phases), 1)
                    per = (len(mm) + nparts - 1) // nparts if mm else 0
                    for p in range(nparts):
                        if p == 0:
                            if i + 1 < len(items):
                                issue_loads(i + 1)
                            if i < len(items) and epi_pre is not None:
                                epi_pre(items[i][0], items[i][1], i % 2)
                        S.cur.extend(mm[p * per:(p + 1) * per])
                        if p < len(phases):
                            S.cur.extend(phases[p])
                        S.step()
                S.require(tok_st)
                S.op("dve", lambda e: e.memset(flush_t[:], 0.0))
                S.step()

        def stage_mod(l):
            with ExitStack() as st:
                cT = sb(st, "cT", [128, 2, 16], F32)
                scB = sb(st, "scB", [128, 2, 16, 128], BF16)
                ones = sb(st, "ones", [128, 128], F32)
                wb = sb(st, "wb_mod", [128, 16, 512], BF16)
                bb = sb(st, "bb_mod", [128, 512], F32)
                mo = sb(st, "mo_mod", [128, 2, 512], F32)
                ps = [pb(st, "psm%d" % i, [128, 512], F32) for i in range(2)]
                S.dma("sp", cT[:], cvec.rearrange("j (kc p) -> p j kc", p=128), allow_slow_non_contiguous=True)
                S.op("dve", lambda e: e.memset(ones[:], 1.0))
                S.step()
                S.op("act", lambda e: e.activation(out=cT[:], in_=cT[:], func=AF.Silu))
                S.step()
                for j in range(2):
                    for kc in range(16):
                        S.op("dve", lambda e, j=j, kc=kc: e.tensor_scalar(
                            out=scB[:, j, kc, :], in0=ones[:], scalar1=cT[:, j, kc:kc + 1], scalar2=None,
                            op0=ALU.mult))
                S.step()
                for nb in range(6 * D // 512):
                    load_w_block(wb, w_mod[l], 16, nb * 512, 512)
                    S.dma("sp", bb[:], b_mod[l:l + 1, nb * 512:(nb + 1) * 512].partition_broadcast(128))
                    S.step()
                    for j in range(2):
                        for kc in range(16):
                            S.op("pe", lambda e, j=j, kc=kc: e.matmul(
                                ps[j][:, :], lhsT=scB[:, j, kc, :], rhs=wb[:, kc, :],
                                start=(kc == 0), stop=(kc == 15)))
                    S.step()
                    for j in range(2):
                        S.op("dve", lambda e, j=j: e.tensor_tensor(out=mo[:, j, :], in0=ps[j][:, :], in1=bb[:],
                                                                  op=ALU.add))
                    S.step()
                    for j in range(2):
                        S.dma("sp", MB[j, :, nb * 512:(nb + 1) * 512], mo[:, j, :])
                    S.step()

        def stage_norm(l, which, router=None):
            i_shift, i_scale = (0, 1) if which == 1 else (3, 4)
            gsrc = norm1_g if which == 1 else norm2_g
            with ExitStack() as st:
                A = sb(st, "nA", [128, 2, D], F32)
                Bt = sb(st, "nB", [128, 2, D], F32)
                gB = sb(st, "ngB", [128, D], F32)
                x = sb(st, "nx", [128, D], F32)
                junk = sb(st, "njunk", [128, D], F32)
                hb = sb(st, "nhb", [128, D], BF16)
                hT = sb(st, "nhT", [128, 16, 128], BF16)
                ss = sb(st, "nss", [128, 4], F32)
                pT = pb(st, "npT", [128, D], BF16)
                for j in range(2):
                    S.dma("sp", A[:, j, :], MB[j, :, i_scale * D:(i_scale + 1) * D])
                    S.dma("sp", Bt[:, j, :], MB[j, :, i_shift * D:(i_shift + 1) * D])
                S.dma("sp", gB[:], gsrc[l:l + 1, :].partition_broadcast(128))
                S.step()
                for j in range(2):
                    S.op("dve", lambda e, j=j: e.scalar_tensor_tensor(
                        out=A[:, j, :], in0=A[:, j, :], scalar=1.0, in1=gB[:], op0=ALU.add, op1=ALU.mult))
                S.step()
                if router is not None:
                    mi = router
                    rw = sb(st, "rw", [128, 16, NE], F32)
                    rbB = sb(st, "rbB", [128, NE], F32)
                    hT32 = sb(st, "hT32", [128, 16, 128], F32)
                    lg = sb(st, "lg", [128, NE], F32)
                    l2 = sb(st, "l2", [128, NE], F32)
                    ex = sb(st, "ex", [128, NE], F32)
                    sm = sb(st, "sm", [128, 8], F32)
                    pT32 = pb(st, "pT32", [128, D], F32)
                    pl = pb(st, "pl", [128, NE], F32)
                    S.dma("sp", rw[:], router_w[mi].rearrange("(kc p) e -> p kc e", p=128))
                    S.dma("sp", rbB[:], router_b[mi:mi + 1, :].partition_broadcast(128))
                    S.step()
                for t in range(NT):
                    j = 1 if t < NCTX // 128 else 0
                    S.dma("sp", x[:], XR[t * 128:(t + 1) * 128, :])
                    S.op("pool", lambda e: e.memset(ss[:], 0.0))
                    S.step()
                    S.op("act", lambda e: e.activation(out=junk[:], in_=x[:], func=AF.Square, accum_out=ss[:, 0:1]))
                    S.step()
                    S.op("dve", lambda e: e.tensor_scalar(out=ss[:, 1:2], in0=ss[:, 0:1], scalar1=1.0 / D,
                                                         scalar2=EPS, op0=ALU.mult, op1=ALU.add))
                    S.step()
                    S.op("act", lambda e: e.activation(out=ss[:, 3:4], in_=ss[:, 1:2], func=AF.Sqrt))
                    S.step()
                    S.op("dve", lambda e: e.reciprocal(out=ss[:, 2:3], in_=ss[:, 3:4]))
                    S.step()
                    S.op("dve", lambda e, j=j: e.scalar_tensor_tensor(
                        out=junk[:], in0=x[:], scalar=ss[:, 2:3], in1=A[:, j, :], op0=ALU.mult, op1=ALU.mult))
                    S.step()
                    S.op("dve", lambda e, j=j: e.tensor_tensor(out=junk[:], in0=junk[:], in1=Bt[:, j, :], op=ALU.add))
                    S.step()
                    S.op("act", lambda e: e.activation(out=hb[:], in_=junk[:], func=AF.Copy))
                    if router is not None:
                        for kc in range(16):
                            S.op("pe", lambda e, kc=kc: e.transpose(
                                out=pT32[:, kc * 128:(kc + 1) * 128], in_=junk[:, kc * 128:(kc + 1) * 128],
                                identity=ident32))
                    S.step()
                    for kc in range(16):
                        S.op("pe", lambda e, kc=kc: e.transpose(
                            out=pT[:, kc * 128:(kc + 1) * 128], in_=hb[:, kc * 128:(kc + 1) * 128],
                            identity=identb[:]))
                    if router is not None:
                        S.op("dve", lambda e: e.tensor_copy(out=hT32[:].rearrange("p a b -> p (a b)"), in_=pT32[:, :]))
                    S.step()
                    S.op("act", lambda e: e.activation(out=hT[:].rearrange("p a b -> p (a b)"), in_=pT[:, :],
                                                       func=AF.Copy))
                    if router is not None:
                        for kc in range(16):
                            S.op("pe", lambda e, kc=kc: e.matmul(pl[:, :], lhsT=hT32[:, kc, :], rhs=rw[:, kc, :],
                                                                start=(kc == 0), stop=(kc == 15)))
                    S.step()
                    S.dma("sp", HT[:, :, t * 128:(t + 1) * 128], hT[:])
                    if router is not None:
                        S.op("dve", lambda e: e.tensor_tensor(out=lg[:], in0=pl[:, :], in1=rbB[:], op=ALU.add))
                        S.step()
                        S.op("dve", lambda e: e.tensor_reduce(out=sm[:, 0:1], in_=lg[:], axis=AX.X, op=ALU.max))
                        S.step()
                        S.op("dve", lambda e: e.tensor_scalar(out=l2[:], in0=lg[:], scalar1=sm[:, 0:1], scalar2=-1e30,
                                                             op0=ALU.is_equal, op1=ALU.mult))
                        S.op("pool", lambda e: e.tensor_scalar(out=sm[:, 1:2], in0=sm[:, 0:1], scalar1=-1.0,
                                                              scalar2=None, op0=ALU.mult))
                        S.step()
                        S.op("dve", lambda e: e.tensor_tensor(out=l2[:], in0=l2[:], in1=lg[:], op=ALU.add))
                        S.op("act", lambda e: e.activation(out=ex[:], in_=lg[:], func=AF.Exp, bias=sm[:, 1:2],
                                                           scale=1.0))
                        S.step()
                        S.op("dve", lambda e: e.tensor_reduce(out=sm[:, 2:3], in_=l2[:], axis=AX.X, op=ALU.max))
                        S.step()
                        S.op("dve", lambda e: e.tensor_scalar(out=l2[:], in0=lg[:], scalar1=sm[:, 2:3], scalar2=None,
                                                             op0=ALU.is_ge))
                        S.step()
                        S.op("dve", lambda e: e.tensor_tensor(out=ex[:], in0=ex[:], in1=l2[:], op=ALU.mult))
                        S.step()
                        S.op("dve", lambda e: e.tensor_reduce(out=sm[:, 3:4], in_=ex[:], axis=AX.X, op=ALU.add))
                        S.step()
                        S.op("dve", lambda e: e.reciprocal(out=sm[:, 4:5], in_=sm[:, 3:4]))
                        S.step()
                        S.op("dve", lambda e: e.tensor_scalar(out=ex[:], in0=ex[:], scalar1=sm[:, 4:5], scalar2=None,
                                                             op0=ALU.mult))
                        S.step()
                        S.dma("sp", GT[t * 128:(t + 1) * 128, :], ex[:])
                    S.step()

        def gelu_tanh(dst, src, tmp):
            S.op("act", lambda e: e.activation(out=tmp, in_=src, func=AF.Square))
            S.step()
            S.op("dve", lambda e: e.tensor_scalar(out=tmp, in0=tmp, scalar1=0.044715, scalar2=1.0,
                                                 op0=ALU.mult, op1=ALU.add))
            S.step()
            S.op("dve", lambda e: e.tensor_tensor(out=tmp, in0=tmp, in1=src, op=ALU.mult))
            S.step()
            S.op("act", lambda e: e.activation(out=tmp, in_=tmp, func=AF.Sigmoid, scale=1.5957691216057308))
            S.step()
            S.op("dve", lambda e: e.tensor_tensor(out=dst, in0=tmp, in1=src, op=ALU.mult))
            S.step()

        def stage_inproj(l):
            with ExitStack() as st:
                o32 = sb(st, "ip_o32", [128, 2, 4, 512], F32)
                o16 = sb(st, "ip_o16", [128, 2, 4, 512], BF16)
                tmp = sb(st, "ip_tmp", [128, 4, 512], F32)

                def epi_tm(ps, nb, gi, slot=0):
                    tok0, ntok = groups[gi]
                    nj = ntok // 128
                    col = nb * 512
                    for j in range(nj):
                        eng = "act" if j % 2 == 0 else "dve"
                        if col < V0:
                            sc = 1.0 if col < K0 else DK ** -0.5
                            S.op("act", lambda e, j=j, sc=sc: e.activation(out=o32[:, slot, j, :], in_=ps[j][:, :],
                                                                         func=AF.Copy, scale=sc))
                        elif col < G0:
                            S.op("act", lambda e, j=j: e.activation(out=o16[:, slot, j, :], in_=ps[j][:, :], func=AF.Copy))
                        elif col < UA0:
                            S.op("act", lambda e, j=j: e.activation(out=o16[:, slot, j, :], in_=ps[j][:, :], func=AF.Silu))
                    if col >= UA0:
                        for j in range(nj):
                            S.op("act", lambda e, j=j: e.activation(out=o32[:, slot, j, :], in_=ps[j][:, :], func=AF.Copy))
                        S.step()
                        if col < VA0:
                            gelu_tanh(o16[:, slot, 0:nj, :], o32[:, slot, 0:nj, :], tmp[:, 0:nj, :])
                        else:
                            S.op("act", lambda e: e.activation(out=tmp[:, 0:nj, :], in_=o32[:, slot, 0:nj, :], func=AF.Square))
                            S.step()
                            S.op("dve", lambda e: e.tensor_scalar(out=tmp[:, 0:nj, :], in0=tmp[:, 0:nj, :],
                                                                 scalar1=0.044715, scalar2=1.0, op0=ALU.mult,
                                                                 op1=ALU.add))
                            S.step()
                            S.op("dve", lambda e: e.tensor_tensor(out=tmp[:, 0:nj, :], in0=tmp[:, 0:nj, :],
                                                                 in1=o32[:, slot, 0:nj, :], op=ALU.mult))
                            S.step()
                            S.op("act", lambda e: e.activation(out=tmp[:, 0:nj, :], in_=tmp[:, 0:nj, :],
                                                               func=AF.Sigmoid, scale=1.5957691216057308))
                            S.step()
                            S.op("dve", lambda e: e.tensor_tensor(out=o32[:, slot, 0:nj, :], in0=tmp[:, 0:nj, :],
                                                                 in1=o32[:, slot, 0:nj, :], op=ALU.mult))
                    S.step()

                    def dst_rows(M, c0):
                        return M[tok0:tok0 + ntok, c0:c0 + 512].rearrange("(j p) n -> p j n", p=128)
                    if col < V0:
                        S.dma_async("sp", dst_rows(QK, col), o32[:, slot, 0:nj, :], tok_st)
                    elif col < G0:
                        S.dma_async("sp", dst_rows(VV, col - V0), o16[:, slot, 0:nj, :], tok_st)
                    elif col < UA0:
                        S.dma_async("sp", dst_rows(SG, col - G0), o16[:, slot, 0:nj, :], tok_st)
                    elif col < VA0:
                        S.dma_async("sp", dst_rows(UU, col - UA0), o16[:, slot, 0:nj, :], tok_st)
                    else:
                        S.dma_async("sp", dst_rows(VA, col - VA0), o32[:, slot, 0:nj, :], tok_st)
                    S.step()

                gemm(HT, 16, w_in[l][:, 0:GA0], GA0, "tm", None, epi_tm, "ipa")

                def epi_fm(ps, nb, gi, slot=0):
                    tok0, ntok = groups[gi]
                    dst = SGA if nb < 4 else SGR
                    for cb in range(4):
                        S.op("act", lambda e, cb=cb: e.activation(out=o16[:, slot, cb, 0:ntok], in_=ps[cb][:, 0:ntok],
                                                                 func=AF.Sigmoid))
                    S.step()
                    S.dma_async("sp", dst[:, (nb % 4) * 4:(nb % 4) * 4 + 4, tok0:tok0 + ntok], o16[:, slot, :, 0:ntok], tok_st)
                    S.step()

                gemm(HT, 16, w_in[l][:, GA0:IN_COLS], IN_COLS - GA0, "fm", None, epi_fm, "ipb")

        def stage_rope(l, dec):
            with ExitStack() as st:
                qk = sb(st, "r_qk", [128, 4096], F32)
                qkb = sb(st, "r_qkb", [128, 4096], BF16)
                t1 = sb(st, "r_t1", [128, 16, 2, 64], F32)
                t2 = sb(st, "r_t2", [128, 16, 2, 64], F32)
                rt = sb(st, "r_rt", [128, 256], F32)
                kfb = sb(st, "r_kfb", [128, 2, 8, 256], BF16)
                qkT = sb(st, "r_qkT", [128, 32, 128], BF16)
                pT = [pb(st, "r_pT%d" % i, [128, 1024], BF16) for i in range(4)]
                for t in range(NT):
                    S.dma("sp", qk[:], QK[t * 128:(t + 1) * 128, :])
                    is_lat = t >= NCTX // 128
                    if is_lat:
                        S.dma("sp", rt[:], rope_t[t - NCTX // 128, :, :])
                    S.step()
                    if is_lat:
                        v5 = qk[:].rearrange("p (a b h f) -> p a b h f", a=16, b=2, h=2, f=64)
                        o5 = qkb[:].rearrange("p (a b h f) -> p a b h f", a=16, b=2, h=2, f=64)
                        x1 = v5[:, :, :, 0, :]
                        x2 = v5[:, :, :, 1, :]
                        cosb = rt[:, 0:128].rearrange("p (b f) -> p b f", b=2).unsqueeze(1).to_broadcast([128, 16, 2, 64])
                        sinb = rt[:, 128:256].rearrange("p (b f) -> p b f", b=2).unsqueeze(1).to_broadcast([128, 16, 2, 64])
                        S.op("dve", lambda e: e.tensor_tensor(out=t1[:], in0=x1, in1=cosb, op=ALU.mult))
                        S.op("pool", lambda e: e.tensor_tensor(out=t2[:], in0=x2, in1=sinb, op=ALU.mult))
                        S.step()
                        S.op("dve", lambda e: e.tensor_tensor(out=o5[:, :, :, 0, :], in0=t1[:], in1=t2[:], op=ALU.subtract))
                        S.step()
                        S.op("dve", lambda e: e.tensor_tensor(out=t1[:], in0=x1, in1=sinb, op=ALU.mult))
                        S.op("pool", lambda e: e.tensor_tensor(out=t2[:], in0=x2, in1=cosb, op=ALU.mult))
                        S.step()
                        S.op("dve", lambda e: e.tensor_tensor(out=o5[:, :, :, 1, :], in0=t1[:], in1=t2[:], op=ALU.add))
                        S.step()
                    else:
                        S.op("act", lambda e: e.activation(out=qkb[:], in_=qk[:], func=AF.Copy))
                        S.step()
                    kv = qkb[:, 2048:4096].rearrange("p (h d) -> p h d", h=8)
                    S.op("dve", lambda e: e.tensor_tensor(out=kfb[:, 0, :, :], in0=kv,
                                                         in1=dec["kdf"][:, :].unsqueeze(2).to_broadcast([128, 8, 256]),
                                                         op=ALU.mult))
                    S.op("pool", lambda e: e.tensor_tensor(out=kfb[:, 1, :, :], in0=kv,
                                                          in1=dec["kdb"][:, :].unsqueeze(2).to_broadcast([128, 8, 256]),
                                                          op=ALU.mult))
                    for b in range(32):
                        S.op("pe", lambda e, b=b: e.transpose(
                            out=pT[b // 8][:, (b % 8) * 128:(b % 8 + 1) * 128], in_=qkb[:, b * 128:(b + 1) * 128],
                            identity=identb[:]))
                    S.step()
                    for i in range(4):
                        S.op("act" if i % 2 == 0 else "dve",
                             (lambda e, i=i: e.activation(out=qkT[:, i * 8:(i + 1) * 8, :].rearrange("p a b -> p (a b)"),
                                                          in_=pT[i][:, :], func=AF.Copy)) if i % 2 == 0 else
                             (lambda e, i=i: e.tensor_copy(out=qkT[:, i * 8:(i + 1) * 8, :].rearrange("p a b -> p (a b)"),
                                                           in_=pT[i][:, :])))
                    S.dma("sp", KFB[t * 128:(t + 1) * 128, :, :], kfb[:].rearrange("p a h d -> p a (h d)"))
                    S.step()
                    S.dma("sp", QKT[:, :, t * 128:(t + 1) * 128], qkT[:])
                    S.step()

        def stage_decays(l, st):
            dec = {}
            ld = sb(st, "d_ld", [128, 16], F32)
            for nm in ["kdf", "kdb", "qdf", "qdb", "cdf", "cdb"]:
                dec[nm] = sb(st, "d_" + nm, [128, 8], F32)
            dec["mask"] = sb(st, "d_mask", [128, 8, 128], F32)
            mb = sb(st, "d_mb", [128, 8, 128], F32)
            S.dma("sp", ld[:], ret_ld[l:l + 1, :].partition_broadcast(128))
            S.step()
            specs = [("kdf", 0, col_127mi), ("kdb", 8, col_i), ("qdf", 0, col_ip1), ("qdb", 8, col_128mi)]
            for nm, off, colv in specs:
                S.op("dve", lambda e, nm=nm, off=off, colv=colv: e.tensor_scalar(
                    out=dec[nm][:], in0=ld[:, off:off + 8], scalar1=colv, scalar2=None, op0=ALU.mult))
            S.op("dve", lambda e: e.tensor_scalar(out=dec["cdf"][:], in0=ld[:, 0:8], scalar1=128.0, scalar2=None,
                                                 op0=ALU.mult))
            S.op("dve", lambda e: e.tensor_scalar(out=dec["cdb"][:], in0=ld[:, 8:16], scalar1=128.0, scalar2=None,
                                                 op0=ALU.mult))
            S.step()
            for nm in ["kdf", "kdb", "qdf", "qdb", "cdf", "cdb"]:
                S.op("act", lambda e, nm=nm: e.activation(out=dec[nm][:], in_=dec[nm][:], func=AF.Exp))
            for h in range(8):
                S.op("act", lambda e, h=h: e.activation(out=dec["mask"][:, h, :], in_=dpos, func=AF.Exp,
                                                       scale=ld[:, h:h + 1]))
                S.op("act", lambda e, h=h: e.activation(out=mb[:, h, :], in_=dneg, func=AF.Exp,
                                                       scale=ld[:, 8 + h:9 + h]))
            S.step()
            S.op("dve", lambda e: e.tensor_tensor(out=dec["mask"][:], in0=dec["mask"][:],
                                                 in1=umask.unsqueeze(1).to_broadcast([128, 8, 128]), op=ALU.mult))
            S.op("pool", lambda e: e.tensor_tensor(out=mb[:], in0=mb[:],
                                                  in1=lmask.unsqueeze(1).to_broadcast([128, 8, 128]), op=ALU.mult))
            S.step()
            S.op("dve", lambda e: e.tensor_tensor(out=dec["mask"][:], in0=dec["mask"][:], in1=mb[:], op=ALU.add))
            S.step()
            return dec

        def stage_retention(l, dec):
            NC_ = NCTX // 128
            for h in range(HEADS):
                with ExitStack() as st:
                    qT = sb(st, "t_qT", [128, 2, T], BF16)
                    kT = sb(st, "t_kT", [128, 2, T], BF16)
                    v = sb(st, "t_v", [128, NT, 512], BF16)
                    kf = sb(st, "t_kf", [128, NT, 2, 256], BF16)
                    Sst = sb(st, "t_S", [128, 2, 2, 512], F32)
                    Sb16 = sb(st, "t_Sb16", [128, 2, 2, 512], BF16)
                    sfb = sb(st, "t_sfb", [128, 2, 2, 512], BF16)
                    pt = sb(st, "t_pt", [128, 128], BF16)
                    o1 = sb(st, "t_o1", [128, 512], F32)
                    sgt = sb(st, "t_sg", [128, 512], BF16)
                    retb = sb(st, "t_retb", [128, 512], BF16)
                    rT = sb(st, "t_rT", [128, 4, 128], BF16)
                    stt = sb(st, "t_stt", [128, 16], F32)
                    ps_u = [pb(st, "t_psu%d" % i, [128, 512], F32) for i in range(4)]
                    ps_o = pb(st, "t_pso", [128, 512], F32)
                    misc = [pb(st, "t_misc%d" % i, [128, 512], F32) for i in range(2)]
                    ps_s2 = [m[:, 0:128] for m in misc]
                    ps_o2 = [ps_o, pb(st, "t_pso1", [128, 512], F32)]
                    ps_t2 = [m[:, 256:512].bitcast(BF16) for m in misc]
                    sfb2 = sb(st, "t_sfb2", [128, 2, 2, 2, 512], BF16)
                    sgt2 = sb(st, "t_sg2", [128, 2, 512], BF16)
                    pt2 = sb(st, "t_pt2", [128, 2, 128], BF16)
                    o12 = sb(st, "t_o12", [128, 2, 512], F32)
                    retb2 = sb(st, "t_retb2", [128, 2, 512], BF16)
                    rT2 = sb(st, "t_rT2", [128, 2, 4, 128], BF16)
                    stt2 = sb(st, "t_stt2", [128, 2, 16], F32)
                    S.dma("sp", qT[:], QKT[:, h * 2:h * 2 + 2, :])
                    S.dma("sp", kT[:], QKT[:, 16 + h * 2:16 + h * 2 + 2, :])
                    for t0 in range(0, NT, 8):
                        t1_ = min(NT, t0 + 8)
                        S.dma("sp", v[:, t0:t1_, :],
                              VV[t0 * 128:t1_ * 128, h * 512:(h + 1) * 512].rearrange("(t p) n -> p t n", p=128))
                        for a_ in range(2):
                            S.dma("sp", kf[:, t0:t1_, a_, :],
                                  KFB[t0 * 128:t1_ * 128, a_, h * 256:(h + 1) * 256].rearrange("(t p) n -> p t n", p=128))
                    S.op("dve", lambda e: e.memset(Sst[:], 0.0))
                    S.step()
                    fwd_order = list(range(NT))
                    bwd_order = list(range(NC_ - 1, -1, -1)) + list(range(NT - 1, NC_ - 1, -1))
                    for cf, cb_ in zip(fwd_order, bwd_order):
                        S.op("act", lambda e: e.activation(out=Sb16[:, 0], in_=Sst[:, 0], func=AF.Copy))
                        S.op("dve", lambda e: e.tensor_copy(out=Sb16[:, 1], in_=Sst[:, 1]))
                        for dc in range(2):
                            S.op("pe", lambda e, dc=dc, cf=cf: e.matmul(
                                ps_u[dc][:, :], lhsT=kf[:, cf, 0, dc * 128:(dc + 1) * 128], rhs=v[:, cf, :],
                                start=True, stop=True))
                            S.op("pe", lambda e, dc=dc, cb_=cb_: e.matmul(
                                ps_u[2 + dc][:, :], lhsT=kf[:, cb_, 1, dc * 128:(dc + 1) * 128], rhs=v[:, cb_, :],
                                start=True, stop=True))
                        S.step()
                        S.dma("sp", ST[cf, 0], Sb16[:, 0])
                        S.dma("sp", ST[cb_, 1], Sb16[:, 1])
                        for dc in range(2):
                            S.op("dve", lambda e, dc=dc: e.scalar_tensor_tensor(
                                out=Sst[:, 0, dc, :], in0=Sst[:, 0, dc, :], scalar=dec["cdf"][:, h:h + 1],
                                in1=ps_u[dc][:, :], op0=ALU.mult, op1=ALU.add))
                            S.op("dve", lambda e, dc=dc: e.scalar_tensor_tensor(
                                out=Sst[:, 1, dc, :], in0=Sst[:, 1, dc, :], scalar=dec["cdb"][:, h:h + 1],
                                in1=ps_u[2 + dc][:, :], op0=ALU.mult, op1=ALU.add))
                        S.step()
                    for c0 in range(0, NT, 2):
                        cl = [c0, c0 + 1] if c0 + 1 < NT else [c0]
                        K2 = range(len(cl))
                        css = [slice(c * 128, (c + 1) * 128) for c in cl]
                        for k in K2:
                            c = cl[k]
                            S.dma("sp", sfb2[:, k, 0], ST[c, 0])
                            S.dma("sp", sfb2[:, k, 1], ST[c, 1])
                            S.dma("sp", sgt2[:, k, :], SG[c * 128:(c + 1) * 128, h * 512:(h + 1) * 512])
                        S.step()
                        for k in K2:
                            cs = css[k]
                            for dc in range(2):
                                S.op("pe", lambda e, dc=dc, k=k, cs=cs: e.matmul(
                                    ps_s2[k][:, :], lhsT=kT[:, dc, cs], rhs=qT[:, dc, cs], start=(dc == 0), stop=(dc == 1)))
                            for dr in range(2):
                                for dc in range(2):
                                    S.op("pe", lambda e, dr=dr, dc=dc, k=k, cs=cs: e.matmul(
                                        ps_u[2 * k + dr][:, :], lhsT=qT[:, dc, cs], rhs=sfb2[:, k, dr, dc, :],
                                        start=(dc == 0), stop=(dc == 1)))
                        S.step()
                        for k in K2:
                            S.op("dve", lambda e, k=k: e.tensor_tensor(out=pt2[:, k, :], in0=ps_s2[k][:, :],
                                                                      in1=dec["mask"][:, h, :], op=ALU.mult))
                        S.step()
                        for k in K2:
                            S.op("pe", lambda e, k=k, c=cl[k]: e.matmul(ps_o2[k][:, :], lhsT=pt2[:, k, :], rhs=v[:, c, :],
                                                                       start=True, stop=True))
                        S.step()
                        for k in K2:
                            S.op("act", lambda e, k=k: e.activation(out=o12[:, k, :], in_=ps_o2[k][:, :], func=AF.Copy))
                        S.step()
                        for k in K2:
                            S.op("dve", lambda e, k=k: e.scalar_tensor_tensor(
                                out=o12[:, k, :], in0=ps_u[2 * k][:, :], scalar=dec["qdf"][:, h:h + 1], in1=o12[:, k, :],
                                op0=ALU.mult, op1=ALU.add))
                        S.step()
                        for k in K2:
                            S.op("dve", lambda e, k=k: e.scalar_tensor_tensor(
                                out=o12[:, k, :], in0=ps_u[2 * k + 1][:, :], scalar=dec["qdb"][:, h:h + 1],
                                in1=o12[:, k, :], op0=ALU.mult, op1=ALU.add))
                        S.step()
                        for k in K2:
                            S.op("dve", lambda e, k=k: e.bn_stats(out=stt2[:, k, 0:6], in_=o12[:, k, :]))
                        S.step()
                        for k in K2:
                            S.op("dve", lambda e, k=k: e.bn_aggr(out=stt2[:, k, 8:10], in_=stt2[:, k, 0:6]))
                        S.step()
                        for k in K2:
                            S.op("dve", lambda e, k=k: e.tensor_scalar(out=stt2[:, k, 11:12], in0=stt2[:, k, 9:10],
                                                                      scalar1=EPS, scalar2=None, op0=ALU.add))
                        S.step()
                        for k in K2:
                            S.op("act", lambda e, k=k: e.activation(out=stt2[:, k, 12:13], in_=stt2[:, k, 11:12],
                                                                    func=AF.Sqrt))
                        S.step()
                        for k in K2:
                            S.op("dve", lambda e, k=k: e.reciprocal(out=stt2[:, k, 10:11], in_=stt2[:, k, 12:13]))
                        S.step()
                        for k in K2:
                            S.op("dve", lambda e, k=k: e.tensor_scalar(
                                out=o12[:, k, :], in0=o12[:, k, :], scalar1=stt2[:, k, 8:9], scalar2=stt2[:, k, 10:11],
                                op0=ALU.subtract, op1=ALU.mult))
                        S.step()
                        for k in K2:
                            S.op("dve" if k == 0 else "pool", lambda e, k=k: e.tensor_tensor(
                                out=retb2[:, k, :], in0=o12[:, k, :], in1=sgt2[:, k, :], op=ALU.mult))
                        S.step()
                        for k in K2:
                            for i in range(4):
                                S.op("pe", lambda e, i=i, k=k: e.transpose(
                                    out=ps_t2[k][:, i * 128:(i + 1) * 128], in_=retb2[:, k, i * 128:(i + 1) * 128],
                                    identity=identb[:]))
                        S.step()
                        for k in K2:
                            if k == 0:
                                S.op("act", lambda e, k=k: e.activation(
                                    out=rT2[:, k].rearrange("p a b -> p (a b)"), in_=ps_t2[k][:, :], func=AF.Copy))
                            else:
                                S.op("dve", lambda e, k=k: e.tensor_copy(
                                    out=rT2[:, k].rearrange("p a b -> p (a b)"), in_=ps_t2[k][:, :]))
                        S.step()
                        for k in K2:
                            S.dma("sp", RETT[:, h * 4:(h + 1) * 4, css[k]], rT2[:, k])
                        S.step()

        def stage_sgu(l):
            with ExitStack() as st:
                w32 = sb(st, "g_w32", [128, 8, 128], F32)
                w16 = sb(st, "g_w16", [128, 8, 128], BF16)
                wsT = sb(st, "g_wsT", [128, 8, 128], BF16)
                bsT = sb(st, "g_bsT", [128, 8], F32)
                lg_ = sb(st, "g_lg", [128, D], F32)
                lb_ = sb(st, "g_lb", [128, D], F32)
                va = sb(st, "g_va", [128, D], F32)
                uu = sb(st, "g_uu", [128, D], BF16)
                vn = sb(st, "g_vn", [128, D], BF16)
                s32 = sb(st, "g_s32", [128, D], F32)
                sT = sb(st, "g_sT", [128, 16, 128], BF16)
                stt = sb(st, "g_stt", [128, 32], F32)
                ps = [pb(st, "g_ps%d" % i, [128, 512], F32) for i in range(4)]
                pT = pb(st, "g_pT", [128, D], BF16)
                S.dma("sp", w32[:], sgu_w[l].rearrange("g p q -> p g q"))
                S.dma("sp", bsT[:], sgu_b[l].rearrange("g p -> p g"), allow_slow_non_contiguous=True)
                S.dma("sp", lg_[:], sgu_ln_g[l:l + 1, :].partition_broadcast(128))
                S.dma("sp", lb_[:], sgu_ln_b[l:l + 1, :].partition_broadcast(128))
                S.step()
                S.op("act", lambda e: e.activation(out=w16[:], in_=w32[:], func=AF.Copy))
                S.step()
                for g in range(8):
                    S.op("pe", lambda e, g=g: e.transpose(out=pT[:, g * 128:(g + 1) * 128], in_=w16[:, g, :],
                                                         identity=identb[:]))
                S.step()
                S.op("act", lambda e: e.activation(out=wsT[:].rearrange("p a b -> p (a b)"), in_=pT[:, 0:1024],
                                                   func=AF.Copy))
                S.step()
                for t in range(NT):
                    S.dma("sp", va[:], VA[t * 128:(t + 1) * 128, :])
                    S.dma("sp", uu[:], UU[t * 128:(t + 1) * 128, :])
                    S.step()
                    for i in range(4):
                        S.op("dve", lambda e, i=i: e.bn_stats(out=stt[:, i * 6:(i + 1) * 6],
                                                             in_=va[:, i * 512:(i + 1) * 512]))
                    S.step()
                    S.op("dve", lambda e: e.bn_aggr(out=stt[:, 24:26], in_=stt[:, 0:24].rearrange("p (a b) -> p a b", b=6)))
                    S.step()
                    S.op("dve", lambda e: e.tensor_scalar(out=stt[:, 27:28], in0=stt[:, 25:26], scalar1=EPS,
                                                         scalar2=None, op0=ALU.add))
                    S.step()
                    S.op("act", lambda e: e.activation(out=stt[:, 28:29], in_=stt[:, 27:28], func=AF.Sqrt))
                    S.step()
                    S.op("dve", lambda e: e.reciprocal(out=stt[:, 26:27], in_=stt[:, 28:29]))
                    S.step()
                    S.op("dve", lambda e: e.tensor_scalar(out=va[:], in0=va[:], scalar1=stt[:, 24:25],
                                                         scalar2=stt[:, 26:27], op0=ALU.subtract, op1=ALU.mult))
                    S.step()
                    S.op("dve", lambda e: e.tensor_tensor(out=va[:], in0=va[:], in1=lg_[:], op=ALU.mult))
                    S.step()
                    S.op("dve", lambda e: e.tensor_tensor(out=vn[:], in0=va[:], in1=lb_[:], op=ALU.add))
                    S.step()
                    for g in range(8):
                        S.op("pe", lambda e, g=g: e.matmul(ps[g // 2][:, (g % 2) * 256:(g % 2 + 1) * 256],
                                                          lhsT=wsT[:, g, :], rhs=vn[:, g * 256:(g + 1) * 256],
                                                          start=True, stop=True))
                    S.step()
                    for g in range(8):
                        S.op("dve", lambda e, g=g: e.tensor_scalar(
                            out=s32[:, g * 256:(g + 1) * 256], in0=ps[g // 2][:, (g % 2) * 256:(g % 2 + 1) * 256],
                            scalar1=bsT[:, g:g + 1], scalar2=None, op0=ALU.add))
                    S.step()
                    S.op("dve", lambda e: e.tensor_tensor(out=vn[:], in0=s32[:], in1=uu[:], op=ALU.mult))
                    S.step()
                    for kc in range(16):
                        S.op("pe", lambda e, kc=kc: e.transpose(out=pT[:, kc * 128:(kc + 1) * 128],
                                                               in_=vn[:, kc * 128:(kc + 1) * 128], identity=identb[:]))
                    S.step()
                    S.op("act", lambda e: e.activation(out=sT[:].rearrange("p a b -> p (a b)"), in_=pT[:, :], func=AF.Copy))
                    S.step()
                    S.dma("sp", SGT[:, :, t * 128:(t + 1) * 128], sT[:])
                    S.step()

        def fm_epilogue(st, tag, func, mul_src, add_src, dst, dst_dt, cb_off=0, grp=None):
            G_ = grp or groups
            m16 = sb(st, tag + "_m", [128, 2, 4, 512], BF16) if mul_src is not None else None
            a32 = sb(st, tag + "_a", [128, 2, 4, 512], F32) if add_src is not None else None
            o = sb(st, tag + "_o", [128, 2, 4, 512], dst_dt)
            t32 = sb(st, tag + "_t", [128, 4, 512], F32)

            def pre(nb, gi, slot):
                tok0, ntok = G_[gi]
                if mul_src is not None:
                    S.dma_async("sp", m16[:, slot, :, 0:ntok],
                                mul_src[:, cb_off + nb * 4:cb_off + nb * 4 + 4, tok0:tok0 + ntok], tok_pre[slot])
                if add_src is not None:
                    S.dma_async("sp", a32[:, slot, :, 0:ntok],
                                add_src[:, cb_off + nb * 4:cb_off + nb * 4 + 4, tok0:tok0 + ntok], tok_pre[slot])

            def epi(ps, nb, gi, slot):
                tok0, ntok = G_[gi]
                if mul_src is None:
                    for cb in range(4):
                        S.op("act", lambda e, cb=cb: e.activation(out=o[:, slot, cb, 0:ntok], in_=ps[cb][:, 0:ntok], func=func))
                    S.step()
                else:
                    tgt = o[:, slot] if add_src is None else t32
                    for cb in range(4):
                        eng = "dve" if cb % 2 == 0 else "pool"
                        if func == AF.Copy:
                            S.op("dve", lambda e, cb=cb, tgt=tgt: e.tensor_tensor(
                                out=tgt[:, cb, 0:ntok], in0=ps[cb][:, 0:ntok], in1=m16[:, slot, cb, 0:ntok], op=ALU.mult))
                        else:
                            S.op("act", lambda e, cb=cb: e.activation(out=t32[:, cb, 0:ntok], in_=ps[cb][:, 0:ntok], func=func))
                    S.step()
                    if func != AF.Copy:
                        S.op("dve", lambda e, tgt=tgt: e.tensor_tensor(out=tgt[:, :, 0:ntok], in0=t32[:, :, 0:ntok],
                                                                      in1=m16[:, slot, :, 0:ntok], op=ALU.mult))
                        S.step()
                    if add_src is not None:
                        S.op("pool", lambda e: e.tensor_tensor(out=o[:, slot, :, 0:ntok], in0=t32[:, :, 0:ntok],
                                                              in1=a32[:, slot, :, 0:ntok], op=ALU.add))
                        S.step()
                S.dma_async("sp", dst[:, cb_off + nb * 4:cb_off + nb * 4 + 4, tok0:tok0 + ntok], o[:, slot, :, 0:ntok], tok_st)
                S.step()
            return (pre if (mul_src is not None or add_src is not None) else None), epi

        def resid_epilogue(st, tag, mb_idx, gate_e=None, grp=None):
            G_ = grp or groups
            cv = sb(st, tag + "_cv", [128, 2, D], F32)
            xb = sb(st, tag + "_xb", [128, 2, 4, 512], F32)
            tm = sb(st, tag + "_tm", [128, 4, 512], F32)
            gt = sb(st, tag + "_gt", [128, 2, 4, NE], F32)
            for j in range(2):
                S.dma("sp", cv[:, j, :], MB[j, :, mb_idx * D:(mb_idx + 1) * D])
            S.step()

            def pre(nb, gi, slot):
                tok0, ntok = G_[gi]
                nj = ntok // 128
                S.dma_async("sp", xb[:, slot, 0:nj, :],
                            XR[tok0:tok0 + ntok, nb * 512:(nb + 1) * 512].rearrange("(j p) n -> p j n", p=128),
                            tok_pre[slot])
                if gate_e is not None:
                    S.dma_async("sp", gt[:, slot, 0:nj, :],
                                GT[tok0:tok0 + ntok, :].rearrange("(j p) n -> p j n", p=128), tok_pre[slot])

            def epi(ps, nb, gi, slot):
                tok0, ntok = G_[gi]
                nj = ntok // 128
                jc = 1 if tok0 < NCTX else 0
                for j in range(nj):
                    if gate_e is None:
                        S.op("dve", lambda e, j=j: e.tensor_tensor(out=tm[:, j, :], in0=ps[j][:, :],
                                                                  in1=cv[:, jc, nb * 512:(nb + 1) * 512], op=ALU.mult))
                    else:
                        S.op("dve", lambda e, j=j: e.scalar_tensor_tensor(
                            out=tm[:, j, :], in0=ps[j][:, :], scalar=gt[:, slot, j, gate_e:gate_e + 1],
                            in1=cv[:, jc, nb * 512:(nb + 1) * 512], op0=ALU.mult, op1=ALU.mult))
                S.step()
                S.op("pool", lambda e: e.tensor_tensor(out=xb[:, slot, 0:nj, :], in0=xb[:, slot, 0:nj, :],
                                                      in1=tm[:, 0:nj, :], op=ALU.add))
                S.step()
                S.dma("sp", XR[tok0:tok0 + ntok, nb * 512:(nb + 1) * 512].rearrange("(j p) n -> p j n", p=128),
                      xb[:, slot, 0:nj, :])
                S.step()
            return pre, epi

        def stage_merge(l):
            with ExitStack() as st:
                pre, epi = fm_epilogue(st, "pa", AF.Copy, SGA, None, YT, F32)
                gemm(SGT, 16, w_proj_a[l], D, "fm", pre, epi, "pa")
            with ExitStack() as st:
                pre, epi = fm_epilogue(st, "pr", AF.Copy, SGR, YT, YTB, BF16)
                gemm(RETT, 32, w_proj_r[l], D, "fm", pre, epi, "pr")
            with ExitStack() as st:
                pre, epi = resid_epilogue(st, "wo", 2)
                gemm(YTB, 16, w_out[l], D, "tm", pre, epi, "wo")

        def stage_ffn_dense(l, di):
            with ExitStack() as st:
                pre, epi = fm_epilogue(st, "f1", AF.Silu, None, None, H1T, BF16)
                gemm(HT, 16, ffn_w1[di], D_FF, "fm", pre, epi, "f1")
            with ExitStack() as st:
                pre, epi = fm_epilogue(st, "f3", AF.Copy, H1T, None, HIDT, BF16)
                gemm(HT, 16, ffn_w3[di], D_FF, "fm", pre, epi, "f3")
            with ExitStack() as st:
                g256 = [(i * 256, 256) for i in range(T // 256)]
                pre, epi = resid_epilogue(st, "f2", 5, grp=g256)
                gemm(HIDT, 44, ffn_w2[di], D, "tm", pre, epi, "f2", grp=g256)

        def stage_ffn_moe(l, mi):
            for e_ in range(NE):
                with ExitStack() as st:
                    pre, epi = fm_epilogue(st, "m1", AF.Silu, None, None, H1T, BF16)
                    gemm(HT, 16, moe_w1[mi, e_], D_FFE, "fm", pre, epi, "m1")
                with ExitStack() as st:
                    pre, epi = fm_epilogue(st, "m3", AF.Copy, H1T, None, HIDT, BF16)
                    gemm(HT, 16, moe_w3[mi, e_], D_FFE, "fm", pre, epi, "m3")
                with ExitStack() as st:
                    pre, epi = resid_epilogue(st, "m2", 5, gate_e=e_)
                    gemm(HIDT, 32, moe_w2[mi, e_], D, "tm", pre, epi, "m2")

        def stage_final():
            with ExitStack() as st:
                gB = sb(st, "fgB", [128, D], F32)
                x = sb(st, "fx", [128, D], F32)
                junk = sb(st, "fjunk", [128, D], F32)
                ss = sb(st, "fss", [128, 4], F32)
                if final_norm:
                    S.dma("sp", gB[:], final_g[0:1, :].partition_broadcast(128))
                    S.step()
                S.dma("sp", x_out.rearrange("(p a) d -> p (a d)", p=128), XR.rearrange("(p a) d -> p (a d)", p=128))
                S.step()
                for t in range(NCTX // 128, NT):
                    S.dma("sp", x[:], XR[t * 128:(t + 1) * 128, :])
                    S.op("pool", lambda e: e.memset(ss[:], 0.0))
                    S.step()
                    if final_norm:
                        S.op("act", lambda e: e.activation(out=junk[:], in_=x[:], func=AF.Square, accum_out=ss[:, 0:1]))
                        S.step()
                        S.op("dve", lambda e: e.tensor_scalar(out=ss[:, 1:2], in0=ss[:, 0:1], scalar1=1.0 / D,
                                                             scalar2=EPS, op0=ALU.mult, op1=ALU.add))
                        S.step()
                        S.op("act", lambda e: e.activation(out=ss[:, 3:4], in_=ss[:, 1:2], func=AF.Sqrt))
                        S.step()
                        S.op("dve", lambda e: e.reciprocal(out=ss[:, 2:3], in_=ss[:, 3:4]))
                        S.step()
                        S.op("dve", lambda e: e.scalar_tensor_tensor(out=x[:], in0=x[:], scalar=ss[:, 2:3], in1=gB[:],
                                                                    op0=ALU.mult, op1=ALU.mult))
                        S.step()
                    S.dma("sp", y_out[t * 128 - NCTX:(t + 1) * 128 - NCTX, :], x[:])
                    S.step()

        di = mi = 0
        for l, kind in enumerate(layer_kinds):
            stage_mod(l)
            stage_norm(l, 1)
            stage_inproj(l)
            with ExitStack() as dst_:
                dec = stage_decays(l, dst_)
                stage_rope(l, dec)
                stage_retention(l, dec)
            stage_sgu(l)
            stage_merge(l)
            if kind == "dense":
                stage_norm(l, 2)
                stage_ffn_dense(l, di)
                di += 1
            else:
                stage_norm(l, 2, router=mi)
                stage_ffn_moe(l, mi)
                mi += 1
        stage_final()
        for sm, v in S.prev:
            nc.sync.wait_ge(sm, v)
    return nc


def make_consts():
    i = np.arange(128, dtype=np.float32)
    d = i[None, :] - i[:, None]
    c = np.zeros((128, 5 * 128 + 8), np.float32)
    c[:, 0:128] = np.eye(128, dtype=np.float32)
    c[:, 128:256] = np.maximum(d, 0)
    c[:, 256:384] = np.maximum(-d, 0)
    c[:, 384:512] = (d >= 0)
    c[:, 512:640] = (d <= 0)
    c[:, 640] = i
    c[:, 641] = 127 - i
    c[:, 642] = i + 1
    c[:, 643] = 128 - i
    return c


def make_rope(n_lat):
    half = 64
    freqs = (10000.0 ** (-np.arange(half, dtype=np.float32) / half)).astype(np.float32)
    t = np.arange(n_lat)
    rows = (t // GRID_W).astype(np.float32)
    cols = (t % GRID_W).astype(np.float32)
    ang_r = rows[:, None] * freqs[None, :]
    ang_c = cols[:, None] * freqs[None, :]
    out = np.zeros((n_lat, 256), np.float32)
    out[:, 0:64] = np.cos(ang_r)
    out[:, 64:128] = np.cos(ang_c)
    out[:, 128:192] = np.sin(ang_r)
    out[:, 192:256] = np.sin(ang_c)
    return out.reshape(n_lat // 128, 128, 256)


_PROGS = {}


def _prog(n_lat, kind):
    key = (n_lat, kind)
    if key not in _PROGS:
        _PROGS[key] = build(n_lat, [kind], final_norm=True)
    return _PROGS[key]


def run(inputs, layer_kinds, final_norm=True, cores=None):
    x = np.asarray(inputs["x"], np.float32)
    B, n_lat, _ = x.shape
    consts = make_consts()
    rope = make_rope(n_lat)
    f = lambda k: np.asarray(inputs[k], np.float32)
    state = [np.concatenate([f("ctx")[b], x[b]], axis=0) for b in range(B)]
    cvecs = [np.stack([f("c")[b], f("c_ctx")], axis=0) for b in range(B)]
    di = mi = 0
    y = None
    for l, kind in enumerate(layer_kinds):
        nc = _prog(n_lat, kind)
        sl = lambda k: np.ascontiguousarray(f(k)[l:l + 1])
        shared = {
            "w_mod": sl("w_mod"), "b_mod": sl("b_mod"), "norm1_g": sl("norm1_g"), "norm2_g": sl("norm2_g"),
            "w_in": sl("w_in"), "sgu_ln_g": sl("sgu_ln_g"), "sgu_ln_b": sl("sgu_ln_b"),
            "sgu_w": sl("sgu_w"), "sgu_b": sl("sgu_b"),
            "ret_log_decay": sl("ret_log_decay").reshape(1, 16),
            "w_proj_a": sl("w_proj_a"), "w_proj_r": sl("w_proj_r"), "w_out": sl("w_out"),
            "final_norm_g": f("final_norm_g").reshape(1, D), "consts": consts, "rope_t": rope,
        }
        ii = di if kind == "dense" else 0
        jj = mi if kind == "moe" else 0
        for k in ["ffn_w1", "ffn_w3", "ffn_w2"]:
            shared[k] = np.ascontiguousarray(f(k)[ii:ii + 1])
        for k in ["router_w", "router_b", "moe_w1", "moe_w3", "moe_w2"]:
            shared[k] = np.ascontiguousarray(f(k)[jj:jj + 1])
        if kind == "dense":
            di += 1
        else:
            mi += 1
        in_maps = []
        for b in range(B):
            m = dict(shared)
            m["xin"] = np.ascontiguousarray(state[b])
            m["cvec"] = cvecs[b]
            in_maps.append(m)
        res = run_bass_kernel_spmd(nc, in_maps, core_ids=list(range(B)))
        state = [np.asarray(res.results[b]["x_out"], np.float32) for b in range(B)]
        y = [np.asarray(res.results[b]["y_out"], np.float32) for b in range(B)]
    return np.stack(y, axis=0)


def kernel(**inputs):
    return run(inputs, ["dense", "moe", "dense", "moe"], True)
```

```python
import math
from contextlib import ExitStack

import numpy as np
import concourse.bass as bass
import concourse.mybir as mybir
from concourse.bass_utils import run_bass_kernel_spmd

F32 = mybir.dt.float32
BF16 = mybir.dt.bfloat16
AF = mybir.ActivationFunctionType
ALU = mybir.AluOpType
AX = mybir.AxisListType

D = 2048
NCTX = 256
GRID_W = 64
HEADS = 8
DK = 256
DV = 512
IN_COLS = 20480
D_FF = 5632
NE = 8
D_FFE = 4096
EPS = 1e-6
Q0, K0, V0, G0, UA0, VA0, GA0, GR0 = 0, 2048, 4096, 8192, 12288, 14336, 16384, 18432


class Sched:
    def __init__(self, nc, es):
        self.nc = nc
        self.es = es
        self.eng = {"pe": nc.tensor, "act": nc.scalar, "dve": nc.vector, "pool": nc.gpsimd, "sp": nc.sync}
        self.sem = {k: es.enter_context(nc.semaphore("s_" + k)) for k in ["pe", "act", "dve", "pool", "dma"]}
        self.cnt = {k: 0 for k in self.sem}
        self.prev = []
        self.extra = []
        self.cur = []
        self.rec = None

    def token(self, name):
        return {"sem": self.es.enter_context(self.nc.semaphore(name)), "cnt": 0}

    def _q(self):
        return self.rec[-1] if self.rec is not None else self.cur

    def op(self, e, fn):
        self._q().append((e, fn, False))

    def dma(self, e, out, in_, **kw):
        self._q().append((e, lambda eng: eng.dma_start(out=out, in_=in_, **kw), True))

    def dma_async(self, e, out, in_, tok, **kw):
        self._q().append((e, lambda eng: eng.dma_start(out=out, in_=in_, **kw), tok))

    def require(self, tok):
        self.extra.append(tok)

    def require_val(self, tok, val):
        self.extra.append({"sem": tok["sem"], "cnt": val})

    def record(self, fn):
        self.rec = [[]]
        fn()
        ph = [p for p in self.rec if p]
        self.rec = None
        return ph

    def step(self):
        if self.rec is not None:
            if self.rec[-1]:
                self.rec.append([])
            return
        if not self.cur:
            return
        by = {}
        for e, fn, d in self.cur:
            by.setdefault(e, []).append((fn, d))
        used = set()
        waits = list(self.prev) + [(t["sem"], t["cnt"]) for t in self.extra]
        for e, lst in by.items():
            eng = self.eng[e]
            for sm, v in waits:
                eng.wait_ge(sm, v)
            last_c = max([i for i, (_, d) in enumerate(lst) if d is False], default=-1)
            for i, (fn, d) in enumerate(lst):
                ins = fn(eng)
                if d is True:
                    ins.then_inc(self.sem["dma"], 16)
                    self.cnt["dma"] += 16
                    used.add("dma")
                elif d is not False:
                    ins.then_inc(d["sem"], 16)
                    d["cnt"] += 16
                elif e == "pool" or i == last_c:
                    ins.then_inc(self.sem[e], 1)
                    self.cnt[e] += 1
                    used.add(e)
        self.prev = [(self.sem[k], self.cnt[k]) for k in used]
        self.extra = []
        self.cur = []


def build(n_lat, layer_kinds, final_norm=True):
    T = NCTX + n_lat
    NT = T // 128
    L = len(layer_kinds)
    n_dense = sum(1 for k in layer_kinds if k == "dense")
    n_moe = L - n_dense
    nc = bass.Bass("TRN2", target_bir_lowering=False)

    def din(name, shape):
        return nc.dram_tensor(name, list(shape), F32, kind="ExternalInput").ap()

    xin = din("xin", [T, D])
    cvec = din("cvec", [2, D])
    w_mod = din("w_mod", [L, D, 6 * D])
    b_mod = din("b_mod", [L, 6 * D])
    norm1_g = din("norm1_g", [L, D])
    norm2_g = din("norm2_g", [L, D])
    w_in = din("w_in", [L, D, IN_COLS])
    sgu_ln_g = din("sgu_ln_g", [L, D])
    sgu_ln_b = din("sgu_ln_b", [L, D])
    sgu_w = din("sgu_w", [L, 8, 128, 128])
    sgu_b = din("sgu_b", [L, 8, 128])
    ret_ld = din("ret_log_decay", [L, 16])
    w_proj_a = din("w_proj_a", [L, D, D])
    w_proj_r = din("w_proj_r", [L, 2 * D, D])
    w_out = din("w_out", [L, D, D])
    ffn_w1 = din("ffn_w1", [max(n_dense, 1), D, D_FF])
    ffn_w3 = din("ffn_w3", [max(n_dense, 1), D, D_FF])
    ffn_w2 = din("ffn_w2", [max(n_dense, 1), D_FF, D])
    router_w = din("router_w", [max(n_moe, 1), D, NE])
    router_b = din("router_b", [max(n_moe, 1), NE])
    moe_w1 = din("moe_w1", [max(n_moe, 1), NE, D, D_FFE])
    moe_w3 = din("moe_w3", [max(n_moe, 1), NE, D, D_FFE])
    moe_w2 = din("moe_w2", [max(n_moe, 1), NE, D_FFE, D])
    final_g = din("final_norm_g", [1, D])
    consts = din("consts", [128, 5 * 128 + 8])
    rope_t = din("rope_t", [max(n_lat // 128, 1), 128, 256])
    y_out = nc.dram_tensor("y_out", [n_lat, D], F32, kind="ExternalOutput").ap()
    x_out = nc.dram_tensor("x_out", [T, D], F32, kind="ExternalOutput").ap()

    def dscr(name, shape, dt):
        return nc.dram_tensor(name, list(shape), dt).ap()

    XR = dscr("XR", [T, D], F32)
    MB = dscr("MB", [2, 128, 6 * D], F32)
    HT = dscr("HT", [128, 16, T], BF16)
    QK = dscr("QK", [T, 4096], F32)
    VV = dscr("VV", [T, 4096], BF16)
    SG = dscr("SG", [T, 4096], BF16)
    UU = dscr("UU", [T, D], BF16)
    VA = dscr("VA", [T, D], F32)
    SGA = dscr("SGA", [128, 16, T], BF16)
    SGR = dscr("SGR", [128, 16, T], BF16)
    QKT = dscr("QKT", [128, 32, T], BF16)
    KFB = dscr("KFB", [T, 2, D], BF16)
    ST = dscr("ST", [NT, 2, 128, 2, 512], BF16)
    RETT = dscr("RETT", [128, 32, T], BF16)
    SGT = dscr("SGT", [128, 16, T], BF16)
    YT = dscr("YT", [128, 16, T], F32)
    YTB = dscr("YTB", [128, 16, T], BF16)
    H1T = dscr("H1T", [128, 44, T], BF16)
    HIDT = dscr("HIDT", [128, 44, T], BF16)
    GT = dscr("GT", [T, NE], F32)

    groups = [(0, NCTX)] + [(NCTX + i * 512, min(512, T - NCTX - i * 512)) for i in range((n_lat + 511) // 512)]

    with ExitStack() as es:
        S = Sched(nc, es)

        uniq = [0]

        def sb(st, name, shape, dt):
            uniq[0] += 1
            return st.enter_context(nc.sbuf_tensor("%s_%d" % (name, uniq[0]), list(shape), dt))

        def pb(st, name, shape, dt):
            uniq[0] += 1
            return st.enter_context(nc.psum_tensor("%s_%d" % (name, uniq[0]), list(shape), dt))

        cst = sb(es, "cst", [128, 5 * 128 + 8], F32)
        identb = sb(es, "identb", [128, 128], BF16)
        S.dma("sp", cst[:], consts[:, :])
        S.dma("pool", identb[:], consts[:, 0:128])
        S.dma("sp", XR.rearrange("(p a) d -> p (a d)", p=128), xin.rearrange("(p a) d -> p (a d)", p=128))
        S.step()
        ident32 = cst[:, 0:128]
        dpos = cst[:, 128:256]
        dneg = cst[:, 256:384]
        umask = cst[:, 384:512]
        lmask = cst[:, 512:640]
        col_i = cst[:, 640:641]
        col_127mi = cst[:, 641:642]
        col_ip1 = cst[:, 642:643]
        col_128mi = cst[:, 643:644]

        tok_at = [S.token("t_at%d" % i) for i in range(2)]
        tok_wb = [S.token("t_wb%d" % i) for i in range(4)]
        tok_pre = [S.token("t_pre%d" % i) for i in range(2)]
        tok_st = S.token("t_st")
        flush_t = sb(es, "flush_t", [128, 8], F32)

        def load_w_block(wb, wsrc, KC, n0, ncols, tok=None):
            for k0 in range(0, KC, 8):
                k1 = min(KC, k0 + 8)
                src = wsrc[k0 * 128:k1 * 128, n0:n0 + ncols].rearrange("(kc p) n -> p kc n", p=128)
                if tok is None:
                    S.dma("pool", wb[:, k0:k1, 0:ncols], src)
                else:
                    S.dma_async("pool", wb[:, k0:k1, 0:ncols], src, tok)

        def gemm(AT, KC, wsrc, N, form, epi_pre, epi, tag, grp=None):
            grp = grp or groups
            atw = max(n for _, n in grp)
            NB = N // 512
            PW = 2 if KC <= 16 else 1
            NWS = 2 * PW
            with ExitStack() as st:
                wb = [sb(st, "wb_" + tag, [128, KC, 512], BF16) for _ in range(NWS)]
                at = [sb(st, "at_" + tag, [128, KC, atw], BF16) for _ in range(2)]
                ps = [[pb(st, "ps%d_%s" % (i, tag), [128, 512], F32) for i in range(4)] for _ in range(2)]
                items = []
                w_issue = {}
                a_idx = -1
                prev_start = None
                for nb0 in range(0, NB, PW):
                    blks = list(range(nb0, min(NB, nb0 + PW)))
                    start = len(items)
                    if prev_start is not None:
                        for nb in blks:
                            w_issue.setdefault(prev_start + 1 + (nb - nb0), []).append(nb)
                    prev_start = start
                    for gi in range(len(grp)):
                        a_idx += 1
                        for h_, nb in enumerate(blks):
                            items.append((nb, gi, h_ == 0, a_idx))
                w_first = {}
                for i_, it in enumerate(items):
                    w_first.setdefault(it[0], i_)

                def issue_loads(i):
                    nb, gi, first, ai = items[i]
                    if first:
                        tok0, ntok = grp[gi]
                        s_ = ai % 2
                        for k0 in range(0, KC, 8):
                            k1 = min(KC, k0 + 8)
                            S.dma_async("sp", at[s_][:, k0:k1, 0:ntok], AT[:, k0:k1, tok0:tok0 + ntok], tok_at[s_])
                    for nbn in w_issue.get(i, []):
                        load_w_block(wb[nbn % NWS], wsrc, KC, nbn * 512, 512, tok_wb[nbn % NWS])

                for nb in range(min(PW, NB)):
                    load_w_block(wb[nb % NWS], wsrc, KC, nb * 512, 512, tok_wb[nb % NWS])
                issue_loads(0)
                S.step()
                hist = []
                for i in range(len(items) + 1):
                    hist.append(tok_st["cnt"])
                    if i >= 1:
                        S.require_val(tok_st, hist[i - 1])
                    phases = []
                    if i >= 1:
                        nb_, gi_ = items[i - 1][0], items[i - 1][1]
                        phases = S.record(lambda: epi(ps[(i - 1) % 2], nb_, gi_, (i - 1) % 2))
                        if epi_pre is not None:
                            S.require(tok_pre[(i - 1) % 2])
                    mm = []
                    if i < len(items):
                        nb, gi, first, ai = items[i]
                        tok0, ntok = grp[gi]
                        a_, w_, p_ = at[ai % 2], wb[nb % NWS], ps[i % 2]
                        if form == "tm":
                            for j in range(ntok // 128):
                                for kc in range(KC):
                                    mm.append(("pe", lambda e, j=j, kc=kc, a_=a_, w_=w_, p_=p_: e.matmul(
                                        p_[j][:, :], lhsT=a_[:, kc, j * 128:(j + 1) * 128], rhs=w_[:, kc, :],
                                        start=(kc == 0), stop=(kc == KC - 1)), False))
                        else:
                            for cb in range(4):
                                for kc in range(KC):
                                    mm.append(("pe", lambda e, cb=cb, kc=kc, a_=a_, w_=w_, p_=p_, ntok=ntok: e.matmul(
                                        p_[cb][:, 0:ntok], lhsT=w_[:, kc, cb * 128:(cb + 1) * 128],
                                        rhs=a_[:, kc, 0:ntok], start=(kc == 0), stop=(kc == KC - 1)), False))
                        if first:
                            S.require(tok_at[ai % 2])
                        if w_first[nb] == i:
                            S.require(tok_wb[nb % NWS])
                    nparts = max(len(phases), 1)
                    per = (len(mm) + nparts - 1) // nparts if mm else 0
                    for p in range(nparts):
                        if p == 0:
                            if i + 1 < len(items):
                                issue_loads(i + 1)
                            if i < len(items) and epi_pre is not None:
                                epi_pre(items[i][0], items[i][1], i % 2)
                        S.cur.extend(mm[p * per:(p + 1) * per])
                        if p < len(phases):
                            S.cur.extend(phases[p])
                        S.step()
                S.require(tok_st)
                S.op("dve", lambda e: e.memset(flush_t[:], 0.0))
                S.step()

        def stage_mod(l):
            with ExitStack() as st:
                cT = sb(st, "cT", [128, 2, 16], F32)
                scB = sb(st, "scB", [128, 2, 16, 128], BF16)
                ones = sb(st, "ones", [128, 128], F32)
                wb = sb(st, "wb_mod", [128, 16, 512], BF16)
                bb = sb(st, "bb_mod", [128, 512], F32)
                mo = sb(st, "mo_mod", [128, 2, 512], F32)
                ps = [pb(st, "psm%d" % i, [128, 512], F32) for i in range(2)]
                S.dma("sp", cT[:], cvec.rearrange("j (kc p) -> p j kc", p=128), allow_slow_non_contiguous=True)
                S.op("dve", lambda e: e.memset(ones[:], 1.0))
                S.step()
                S.op("act", lambda e: e.activation(out=cT[:], in_=cT[:], func=AF.Silu))
                S.step()
                for j in range(2):
                    for kc in range(16):
                        S.op("dve", lambda e, j=j, kc=kc: e.tensor_scalar(
                            out=scB[:, j, kc, :], in0=ones[:], scalar1=cT[:, j, kc:kc + 1], scalar2=None,
                            op0=ALU.mult))
                S.step()
                for nb in range(6 * D // 512):
                    load_w_block(wb, w_mod[l], 16, nb * 512, 512)
                    S.dma("sp", bb[:], b_mod[l:l + 1, nb * 512:(nb + 1) * 512].partition_broadcast(128))
                    S.step()
                    for j in range(2):
                        for kc in range(16):
                            S.op("pe", lambda e, j=j, kc=kc: e.matmul(
                                ps[j][:, :], lhsT=scB[:, j, kc, :], rhs=wb[:, kc, :],
                                start=(kc == 0), stop=(kc == 15)))
                    S.step()
                    for j in range(2):
                        S.op("dve", lambda e, j=j: e.tensor_tensor(out=mo[:, j, :], in0=ps[j][:, :], in1=bb[:],
                                                                  op=ALU.add))
                    S.step()
                    for j in range(2):
                        S.dma("sp", MB[j, :, nb * 512:(nb + 1) * 512], mo[:, j, :])
                    S.step()

        def stage_norm(l, which, router=None):
            i_shift, i_scale = (0, 1) if which == 1 else (3, 4)
            gsrc = norm1_g if which == 1 else norm2_g
            with ExitStack() as st:
                A = sb(st, "nA", [128, 2, D], F32)
                Bt = sb(st, "nB", [128, 2, D], F32)
                gB = sb(st, "ngB", [128, D], F32)
                x = sb(st, "nx", [128, D], F32)
                junk = sb(st, "njunk", [128, D], F32)
                hb = sb(st, "nhb", [128, D], BF16)
                hT = sb(st, "nhT", [128, 16, 128], BF16)
                ss = sb(st, "nss", [128, 4], F32)
                pT = pb(st, "npT", [128, D], BF16)
                for j in range(2):
                    S.dma("sp", A[:, j, :], MB[j, :, i_scale * D:(i_scale + 1) * D])
                    S.dma("sp", Bt[:, j, :], MB[j, :, i_shift * D:(i_shift + 1) * D])
                S.dma("sp", gB[:], gsrc[l:l + 1, :].partition_broadcast(128))
                S.step()
                for j in range(2):
                    S.op("dve", lambda e, j=j: e.scalar_tensor_tensor(
                        out=A[:, j, :], in0=A[:, j, :], scalar=1.0, in1=gB[:], op0=ALU.add, op1=ALU.mult))
                S.step()
                if router is not None:
                    mi = router
                    rw = sb(st, "rw", [128, 16, NE], F32)
                    rbB = sb(st, "rbB", [128, NE], F32)
                    hT32 = sb(st, "hT32", [128, 16, 128], F32)
                    lg = sb(st, "lg", [128, NE], F32)
                    l2 = sb(st, "l2", [128, NE], F32)
                    ex = sb(st, "ex", [128, NE], F32)
                    sm = sb(st, "sm", [128, 8], F32)
                    pT32 = pb(st, "pT32", [128, D], F32)
                    pl = pb(st, "pl", [128, NE], F32)
                    S.dma("sp", rw[:], router_w[mi].rearrange("(kc p) e -> p kc e", p=128))
                    S.dma("sp", rbB[:], router_b[mi:mi + 1, :].partition_broadcast(128))
                    S.step()
                for t in range(NT):
                    j = 1 if t < NCTX // 128 else 0
                    S.dma("sp", x[:], XR[t * 128:(t + 1) * 128, :])
                    S.op("pool", lambda e: e.memset(ss[:], 0.0))
                    S.step()
                    S.op("act", lambda e: e.activation(out=junk[:], in_=x[:], func=AF.Square, accum_out=ss[:, 0:1]))
                    S.step()
                    S.op("dve", lambda e: e.tensor_scalar(out=ss[:, 1:2], in0=ss[:, 0:1], scalar1=1.0 / D,
                                                         scalar2=EPS, op0=ALU.mult, op1=ALU.add))
                    S.step()
                    S.op("act", lambda e: e.activation(out=ss[:, 3:4], in_=ss[:, 1:2], func=AF.Sqrt))
                    S.step()
                    S.op("dve", lambda e: e.reciprocal(out=ss[:, 2:3], in_=ss[:, 3:4]))
                    S.step()
                    S.op("dve", lambda e, j=j: e.scalar_tensor_tensor(
                        out=junk[:], in0=x[:], scalar=ss[:, 2:3], in1=A[:, j, :], op0=ALU.mult, op1=ALU.mult))
                    S.step()
                    S.op("dve", lambda e, j=j: e.tensor_tensor(out=junk[:], in0=junk[:], in1=Bt[:, j, :], op=ALU.add))
                    S.step()
                    S.op("act", lambda e: e.activation(out=hb[:], in_=junk[:], func=AF.Copy))
                    if router is not None:
                        for kc in range(16):
                            S.op("pe", lambda e, kc=kc: e.transpose(
                                out=pT32[:, kc * 128:(kc + 1) * 128], in_=junk[:, kc * 128:(kc + 1) * 128],
                                identity=ident32))
                    S.step()
                    for kc in range(16):
                        S.op("pe", lambda e, kc=kc: e.transpose(
                            out=pT[:, kc * 128:(kc + 1) * 128], in_=hb[:, kc * 128:(kc + 1) * 128],
                            identity=identb[:]))
                    if router is not None:
                        S.op("dve", lambda e: e.tensor_copy(out=hT32[:].rearrange("p a b -> p (a b)"), in_=pT32[:, :]))
                    S.step()
                    S.op("act", lambda e: e.activation(out=hT[:].rearrange("p a b -> p (a b)"), in_=pT[:, :],
                                                       func=AF.Copy))
                    if router is not None:
                        for kc in range(16):
                            S.op("pe", lambda e, kc=kc: e.matmul(pl[:, :], lhsT=hT32[:, kc, :], rhs=rw[:, kc, :],
                                                                start=(kc == 0), stop=(kc == 15)))
                    S.step()
                    S.dma("sp", HT[:, :, t * 128:(t + 1) * 128], hT[:])
                    if router is not None:
                        S.op("dve", lambda e: e.tensor_tensor(out=lg[:], in0=pl[:, :], in1=rbB[:], op=ALU.add))
                        S.step()
                        S.op("dve", lambda e: e.tensor_reduce(out=sm[:, 0:1], in_=lg[:], axis=AX.X, op=ALU.max))
                        S.step()
                        S.op("dve", lambda e: e.tensor_scalar(out=l2[:], in0=lg[:], scalar1=sm[:, 0:1], scalar2=-1e30,
                                                             op0=ALU.is_equal, op1=ALU.mult))
                        S.op("pool", lambda e: e.tensor_scalar(out=sm[:, 1:2], in0=sm[:, 0:1], scalar1=-1.0,
                                                              scalar2=None, op0=ALU.mult))
                        S.step()
                        S.op("dve", lambda e: e.tensor_tensor(out=l2[:], in0=l2[:], in1=lg[:], op=ALU.add))
                        S.op("act", lambda e: e.activation(out=ex[:], in_=lg[:], func=AF.Exp, bias=sm[:, 1:2],
                                                           scale=1.0))
                        S.step()
                        S.op("dve", lambda e: e.tensor_reduce(out=sm[:, 2:3], in_=l2[:], axis=AX.X, op=ALU.max))
                        S.step()
                        S.op("dve", lambda e: e.tensor_scalar(out=l2[:], in0=lg[:], scalar1=sm[:, 2:3], scalar2=None,
                                                             op0=ALU.is_ge))
                        S.step()
                        S.op("dve", lambda e: e.tensor_tensor(out=ex[:], in0=ex[:], in1=l2[:], op=ALU.mult))
                        S.step()
                        S.op("dve", lambda e: e.tensor_reduce(out=sm[:, 3:4], in_=ex[:], axis=AX.X, op=ALU.add))
                        S.step()
                        S.op("dve", lambda e: e.reciprocal(out=sm[:, 4:5], in_=sm[:, 3:4]))
                        S.step()
                        S.op("dve", lambda e: e.tensor_scalar(out=ex[:], in0=ex[:], scalar1=sm[:, 4:5], scalar2=None,
                                                             op0=ALU.mult))
                        S.step()
                        S.dma("sp", GT[t * 128:(t + 1) * 128, :], ex[:])
                    S.step()

        def gelu_tanh(dst, src, tmp):
            S.op("act", lambda e: e.activation(out=tmp, in_=src, func=AF.Square))
            S.step()
            S.op("dve", lambda e: e.tensor_scalar(out=tmp, in0=tmp, scalar1=0.044715, scalar2=1.0,
                                                 op0=ALU.mult, op1=ALU.add))
            S.step()
            S.op("dve", lambda e: e.tensor_tensor(out=tmp, in0=tmp, in1=src, op=ALU.mult))
            S.step()
            S.op("act", lambda e: e.activation(out=tmp, in_=tmp, func=AF.Sigmoid, scale=1.5957691216057308))
            S.step()
            S.op("dve", lambda e: e.tensor_tensor(out=dst, in0=tmp, in1=src, op=ALU.mult))
            S.step()

        def stage_inproj(l):
            with ExitStack() as st:
                o32 = sb(st, "ip_o32", [128, 2, 4, 512], F32)
                o16 = sb(st, "ip_o16", [128, 2, 4, 512], BF16)
                tmp = sb(st, "ip_tmp", [128, 4, 512], F32)

                def epi_tm(ps, nb, gi, slot=0):
                    tok0, ntok = groups[gi]
                    nj = ntok // 128
                    col = nb * 512
                    for j in range(nj):
                        eng = "act" if j % 2 == 0 else "dve"
                        if col < V0:
                            sc = 1.0 if col < K0 else DK ** -0.5
                            S.op("act", lambda e, j=j, sc=sc: e.activation(out=o32[:, slot, j, :], in_=ps[j][:, :],
                                                                         func=AF.Copy, scale=sc))
                        elif col < G0:
                            S.op("act", lambda e, j=j: e.activation(out=o16[:, slot, j, :], in_=ps[j][:, :], func=AF.Copy))
                        elif col < UA0:
                            S.op("act", lambda e, j=j: e.activation(out=o16[:, slot, j, :], in_=ps[j][:, :], func=AF.Silu))
                    if col >= UA0:
                        for j in range(nj):
                            S.op("act", lambda e, j=j: e.activation(out=o32[:, slot, j, :], in_=ps[j][:, :], func=AF.Copy))
                        S.step()
                        if col < VA0:
                            gelu_tanh(o16[:, slot, 0:nj, :], o32[:, slot, 0:nj, :], tmp[:, 0:nj, :])
                        else:
                            S.op("act", lambda e: e.activation(out=tmp[:, 0:nj, :], in_=o32[:, slot, 0:nj, :], func=AF.Square))
                            S.step()
                            S.op("dve", lambda e: e.tensor_scalar(out=tmp[:, 0:nj, :], in0=tmp[:, 0:nj, :],
                                                                 scalar1=0.044715, scalar2=1.0, op0=ALU.mult,
                                                                 op1=ALU.add))
                            S.step()
                            S.op("dve", lambda e: e.tensor_tensor(out=tmp[:, 0:nj, :], in0=tmp[:, 0:nj, :],
                                                                 in1=o32[:, slot, 0:nj, :], op=ALU.mult))
                            S.step()
                            S.op("act", lambda e: e.activation(out=tmp[:, 0:nj, :], in_=tmp[:, 0:nj, :],
                                                               func=AF.Sigmoid, scale=1.5957691216057308))
                            S.step()
                            S.op("dve", lambda e: e.tensor_tensor(out=o32[:, slot, 0:nj, :], in0=tmp[:, 0:nj, :],
                                                                 in1=o32[:, slot, 0:nj, :], op=ALU.mult))
                    S.step()

                    def dst_rows(M, c0):
                        return M[tok0:tok0 + ntok, c0:c0 + 512].rearrange("(j p) n -> p j n", p=128)
                    if col < V0:
                        S.dma_async("sp", dst_rows(QK, col), o32[:, slot, 0:nj, :], tok_st)
                    elif col < G0:
                        S.dma_async("sp", dst_rows(VV, col - V0), o16[:, slot, 0:nj, :], tok_st)
                    elif col < UA0:
                        S.dma_async("sp", dst_rows(SG, col - G0), o16[:, slot, 0:nj, :], tok_st)
                    elif col < VA0:
                        S.dma_async("sp", dst_rows(UU, col - UA0), o16[:, slot, 0:nj, :], tok_st)
                    else:
                        S.dma_async("sp", dst_rows(VA, col - VA0), o32[:, slot, 0:nj, :], tok_st)
                    S.step()

                gemm(HT, 16, w_in[l][:, 0:GA0], GA0, "tm", None, epi_tm, "ipa")

                def epi_fm(ps, nb, gi, slot=0):
                    tok0, ntok = groups[gi]
                    dst = SGA if nb < 4 else SGR
                    for cb in range(4):
                        S.op("act", lambda e, cb=cb: e.activation(out=o16[:, slot, cb, 0:ntok], in_=ps[cb][:, 0:ntok],
                                                                 func=AF.Sigmoid))
                    S.step()
                    S.dma_async("sp", dst[:, (nb % 4) * 4:(nb % 4) * 4 + 4, tok0:tok0 + ntok], o16[:, slot, :, 0:ntok], tok_st)
                    S.step()

                gemm(HT, 16, w_in[l][:, GA0:IN_COLS], IN_COLS - GA0, "fm", None, epi_fm, "ipb")

        def stage_rope(l, dec):
            with ExitStack() as st:
                qk = sb(st, "r_qk", [128, 4096], F32)
                qkb = sb(st, "r_qkb", [128, 4096], BF16)
                t1 = sb(st, "r_t1", [128, 16, 2, 64], F32)
                t2 = sb(st, "r_t2", [128, 16, 2, 64], F32)
                rt = sb(st, "r_rt", [128, 256], F32)
                kfb = sb(st, "r_kfb", [128, 2, 8, 256], BF16)
                qkT = sb(st, "r_qkT", [128, 32, 128], BF16)
                pT = [pb(st, "r_pT%d" % i, [128, 1024], BF16) for i in range(4)]
                for t in range(NT):
                    S.dma("sp", qk[:], QK[t * 128:(t + 1) * 128, :])
                    is_lat = t >= NCTX // 128
                    if is_lat:
                        S.dma("sp", rt[:], rope_t[t - NCTX // 128, :, :])
                    S.step()
                    if is_lat:
                        v5 = qk[:].rearrange("p (a b h f) -> p a b h f", a=16, b=2, h=2, f=64)
                        o5 = qkb[:].rearrange("p (a b h f) -> p a b h f", a=16, b=2, h=2, f=64)
                        x1 = v5[:, :, :, 0, :]
                        x2 = v5[:, :, :, 1, :]
                        cosb = rt[:, 0:128].rearrange("p (b f) -> p b f", b=2).unsqueeze(1).to_broadcast([128, 16, 2, 64])
                        sinb = rt[:, 128:256].rearrange("p (b f) -> p b f", b=2).unsqueeze(1).to_broadcast([128, 16, 2, 64])
                        S.op("dve", lambda e: e.tensor_tensor(out=t1[:], in0=x1, in1=cosb, op=ALU.mult))
                        S.op("pool", lambda e: e.tensor_tensor(out=t2[:], in0=x2, in1=sinb, op=ALU.mult))
                        S.step()
                        S.op("dve", lambda e: e.tensor_tensor(out=o5[:, :, :, 0, :], in0=t1[:], in1=t2[:], op=ALU.subtract))
                        S.step()
                        S.op("dve", lambda e: e.tensor_tensor(out=t1[:], in0=x1, in1=sinb, op=ALU.mult))
                        S.op("pool", lambda e: e.tensor_tensor(out=t2[:], in0=x2, in1=cosb, op=ALU.mult))
                        S.step()
                        S.op("dve", lambda e: e.tensor_tensor(out=o5[:, :, :, 1, :], in0=t1[:], in1=t2[:], op=ALU.add))
                        S.step()
                    else:
                        S.op("act", lambda e: e.activation(out=qkb[:], in_=qk[:], func=AF.Copy))
                        S.step()
                    kv = qkb[:, 2048:4096].rearrange("p (h d) -> p h d", h=8)
                    S.op("dve", lambda e: e.tensor_tensor(out=kfb[:, 0, :, :], in0=kv,
                                                         in1=dec["kdf"][:, :].unsqueeze(2).to_broadcast([128, 8, 256]),
                                                         op=ALU.mult))
                    S.op("pool", lambda e: e.tensor_tensor(out=kfb[:, 1, :, :], in0=kv,
                                                          in1=dec["kdb"][:, :].unsqueeze(2).to_broadcast([128, 8, 256]),
                                                          op=ALU.mult))
                    for b in range(32):
                        S.op("pe", lambda e, b=b: e.transpose(
                            out=pT[b // 8][:, (b % 8) * 128:(b % 8 + 1) * 128], in_=qkb[:, b * 128:(b + 1) * 128],
                            identity=identb[:]))
                    S.step()
                    for i in range(4):
                        S.op("act" if i % 2 == 0 else "dve",
                             (lambda e, i=i: e.activation(out=qkT[:, i * 8:(i + 1) * 8, :].rearrange("p a b -> p (a b)"),
                                                          in_=pT[i][:, :], func=AF.Copy)) if i % 2 == 0 else
                             (lambda e, i=i: e.tensor_copy(out=qkT[:, i * 8:(i + 1) * 8, :].rearrange("p a b -> p (a b)"),
                                                           in_=pT[i][:, :])))
                    S.dma("sp", KFB[t * 128:(t + 1) * 128, :, :], kfb[:].rearrange("p a h d -> p a (h d)"))
                    S.step()
                    S.dma("sp", QKT[:, :, t * 128:(t + 1) * 128], qkT[:])
                    S.step()

        def stage_decays(l, st):
            dec = {}
            ld = sb(st, "d_ld", [128, 16], F32)
            for nm in ["kdf", "kdb", "qdf", "qdb", "cdf", "cdb"]:
                dec[nm] = sb(st, "d_" + nm, [128, 8], F32)
            dec["mask"] = sb(st, "d_mask", [128, 8, 128], F32)
            mb = sb(st, "d_mb", [128, 8, 128], F32)
            S.dma("sp", ld[:], ret_ld[l:l + 1, :].partition_broadcast(128))
            S.step()
            specs = [("kdf", 0, col_127mi), ("kdb", 8, col_i), ("qdf", 0, col_ip1), ("qdb", 8, col_128mi)]
            for nm, off, colv in specs:
                S.op("dve", lambda e, nm=nm, off=off, colv=colv: e.tensor_scalar(
                    out=dec[nm][:], in0=ld[:, off:off + 8], scalar1=colv, scalar2=None, op0=ALU.mult))
            S.op("dve", lambda e: e.tensor_scalar(out=dec["cdf"][:], in0=ld[:, 0:8], scalar1=128.0, scalar2=None,
                                                 op0=ALU.mult))
            S.op("dve", lambda e: e.tensor_scalar(out=dec["cdb"][:], in0=ld[:, 8:16], scalar1=128.0, scalar2=None,
                                                 op0=ALU.mult))
            S.step()
            for nm in ["kdf", "kdb", "qdf", "qdb", "cdf", "cdb"]:
                S.op("act", lambda e, nm=nm: e.activation(out=dec[nm][:], in_=dec[nm][:], func=AF.Exp))
            for h in range(8):
                S.op("act", lambda e, h=h: e.activation(out=dec["mask"][:, h, :], in_=dpos, func=AF.Exp,
                                                       scale=ld[:, h:h + 1]))
                S.op("act", lambda e, h=h: e.activation(out=mb[:, h, :], in_=dneg, func=AF.Exp,
                                                       scale=ld[:, 8 + h:9 + h]))
            S.step()
            S.op("dve", lambda e: e.tensor_tensor(out=dec["mask"][:], in0=dec["mask"][:],
                                                 in1=umask.unsqueeze(1).to_broadcast([128, 8, 128]), op=ALU.mult))
            S.op("pool", lambda e: e.tensor_tensor(out=mb[:], in0=mb[:],
                                                  in1=lmask.unsqueeze(1).to_broadcast([128, 8, 128]), op=ALU.mult))
            S.step()
            S.op("dve", lambda e: e.tensor_tensor(out=dec["mask"][:], in0=dec["mask"][:], in1=mb[:], op=ALU.add))
            S.step()
            return dec

        def stage_retention(l, dec):
            NC_ = NCTX // 128
            for h in range(HEADS):
                with ExitStack() as st:
                    qT = sb(st, "t_qT", [128, 2, T], BF16)
                    kT = sb(st, "t_kT", [128, 2, T], BF16)
                    v = sb(st, "t_v", [128, NT, 512], BF16)
                    kf = sb(st, "t_kf", [128, NT, 2, 256], BF16)
                    Sst = sb(st, "t_S", [128, 2, 2, 512], F32)
                    Sb16 = sb(st, "t_Sb16", [128, 2, 2, 512], BF16)
                    sfb = sb(st, "t_sfb", [128, 2, 2, 512], BF16)
                    pt = sb(st, "t_pt", [128, 128], BF16)
                    o1 = sb(st, "t_o1", [128, 512], F32)
                    sgt = sb(st, "t_sg", [128, 512], BF16)
                    retb = sb(st, "t_retb", [128, 512], BF16)
                    rT = sb(st, "t_rT", [128, 4, 128], BF16)
                    stt = sb(st, "t_stt", [128, 16], F32)
                    ps_u = [pb(st, "t_psu%d" % i, [128, 512], F32) for i in range(4)]
                    ps_o = pb(st, "t_pso", [128, 512], F32)
                    misc = [pb(st, "t_misc%d" % i, [128, 512], F32) for i in range(2)]
                    ps_s2 = [m[:, 0:128] for m in misc]
                    ps_o2 = [ps_o, pb(st, "t_pso1", [128, 512], F32)]
                    ps_t2 = [m[:, 256:512].bitcast(BF16) for m in misc]
                    sfb2 = sb(st, "t_sfb2", [128, 2, 2, 2, 512], BF16)
                    sgt2 = sb(st, "t_sg2", [128, 2, 512], BF16)
                    pt2 = sb(st, "t_pt2", [128, 2, 128], BF16)
                    o12 = sb(st, "t_o12", [128, 2, 512], F32)
                    retb2 = sb(st, "t_retb2", [128, 2, 512], BF16)
                    rT2 = sb(st, "t_rT2", [128, 2, 4, 128], BF16)
                    stt2 = sb(st, "t_stt2", [128, 2, 16], F32)
                    S.dma("sp", qT[:], QKT[:, h * 2:h * 2 + 2, :])
                    S.dma("sp", kT[:], QKT[:, 16 + h * 2:16 + h * 2 + 2, :])
                    for t0 in range(0, NT, 8):
                        t1_ = min(NT, t0 + 8)
                        S.dma("sp", v[:, t0:t1_, :],
                              VV[t0 * 128:t1_ * 128, h * 512:(h + 1) * 512].rearrange("(t p) n -> p t n", p=128))
                        for a_ in range(2):
                            S.dma("sp", kf[:, t0:t1_, a_, :],
                                  KFB[t0 * 128:t1_ * 128, a_, h * 256:(h + 1) * 256].rearrange("(t p) n -> p t n", p=128))
                    S.op("dve", lambda e: e.memset(Sst[:], 0.0))
                    S.step()
                    fwd_order = list(range(NT))
                    bwd_order = list(range(NC_ - 1, -1, -1)) + list(range(NT - 1, NC_ - 1, -1))
                    for cf, cb_ in zip(fwd_order, bwd_order):
                        S.op("act", lambda e: e.activation(out=Sb16[:, 0], in_=Sst[:, 0], func=AF.Copy))
                        S.op("dve", lambda e: e.tensor_copy(out=Sb16[:, 1], in_=Sst[:, 1]))
                        for dc in range(2):
                            S.op("pe", lambda e, dc=dc, cf=cf: e.matmul(
                                ps_u[dc][:, :], lhsT=kf[:, cf, 0, dc * 128:(dc + 1) * 128], rhs=v[:, cf, :],
                                start=True, stop=True))
                            S.op("pe", lambda e, dc=dc, cb_=cb_: e.matmul(
                                ps_u[2 + dc][:, :], lhsT=kf[:, cb_, 1, dc * 128:(dc + 1) * 128], rhs=v[:, cb_, :],
                                start=True, stop=True))
                        S.step()
                        S.dma("sp", ST[cf, 0], Sb16[:, 0])
                        S.dma("sp", ST[cb_, 1], Sb16[:, 1])
                        for dc in range(2):
                            S.op("dve", lambda e, dc=dc: e.scalar_tensor_tensor(
                                out=Sst[:, 0, dc, :], in0=Sst[:, 0, dc, :], scalar=dec["cdf"][:, h:h + 1],
                                in1=ps_u[dc][:, :], op0=ALU.mult, op1=ALU.add))
                            S.op("dve", lambda e, dc=dc: e.scalar_tensor_tensor(
                                out=Sst[:, 1, dc, :], in0=Sst[:, 1, dc, :], scalar=dec["cdb"][:, h:h + 1],
                                in1=ps_u[2 + dc][:, :], op0=ALU.mult, op1=ALU.add))
                        S.step()
                    for c0 in range(0, NT, 2):
                        cl = [c0, c0 + 1] if c0 + 1 < NT else [c0]
                        K2 = range(len(cl))
                        css = [slice(c * 128, (c + 1) * 128) for c in cl]
                        for k in K2:
                            c = cl[k]
                            S.dma("sp", sfb2[:, k, 0], ST[c, 0])
                            S.dma("sp", sfb2[:, k, 1], ST[c, 1])
                            S.dma("sp", sgt2[:, k, :], SG[c * 128:(c + 1) * 128, h * 512:(h + 1) * 512])
                        S.step()
                        for k in K2:
                            cs = css[k]
                            for dc in range(2):
                                S.op("pe", lambda e, dc=dc, k=k, cs=cs: e.matmul(
                                    ps_s2[k][:, :], lhsT=kT[:, dc, cs], rhs=qT[:, dc, cs], start=(dc == 0), stop=(dc == 1)))
                            for dr in range(2):
                                for dc in range(2):
                                    S.op("pe", lambda e, dr=dr, dc=dc, k=k, cs=cs: e.matmul(
                                        ps_u[2 * k + dr][:, :], lhsT=qT[:, dc, cs], rhs=sfb2[:, k, dr, dc, :],
                                        start=(dc == 0), stop=(dc == 1)))
                        S.step()
                        for k in K2:
                            S.op("dve", lambda e, k=k: e.tensor_tensor(out=pt2[:, k, :], in0=ps_s2[k][:, :],
                                                                      in1=dec["mask"][:, h, :], op=ALU.mult))
                        S.step()
                        for k in K2:
                            S.op("pe", lambda e, k=k, c=cl[k]: e.matmul(ps_o2[k][:, :], lhsT=pt2[:, k, :], rhs=v[:, c, :],
                                                                       start=True, stop=True))
                        S.step()
                        for k in K2:
                            S.op("act", lambda e, k=k: e.activation(out=o12[:, k, :], in_=ps_o2[k][:, :], func=AF.Copy))
                        S.step()
                        for k in K2:
                            S.op("dve", lambda e, k=k: e.scalar_tensor_tensor(
                                out=o12[:, k, :], in0=ps_u[2 * k][:, :], scalar=dec["qdf"][:, h:h + 1], in1=o12[:, k, :],
                                op0=ALU.mult, op1=ALU.add))
                        S.step()
                        for k in K2:
                            S.op("dve", lambda e, k=k: e.scalar_tensor_tensor(
                                out=o12[:, k, :], in0=ps_u[2 * k + 1][:, :], scalar=dec["qdb"][:, h:h + 1],
                                in1=o12[:, k, :], op0=ALU.mult, op1=ALU.add))
                        S.step()
                        for k in K2:
                            S.op("dve", lambda e, k=k: e.bn_stats(out=stt2[:, k, 0:6], in_=o12[:, k, :]))
                        S.step()
                        for k in K2:
                            S.op("dve", lambda e, k=k: e.bn_aggr(out=stt2[:, k, 8:10], in_=stt2[:, k, 0:6]))
                        S.step()
                        for k in K2:
                            S.op("dve", lambda e, k=k: e.tensor_scalar(out=stt2[:, k, 11:12], in0=stt2[:, k, 9:10],
                                                                      scalar1=EPS, scalar2=None, op0=ALU.add))
                        S.step()
                        for k in K2:
                            S.op("act", lambda e, k=k: e.activation(out=stt2[:, k, 12:13], in_=stt2[:, k, 11:12],
                                                                    func=AF.Sqrt))
                        S.step()
                        for k in K2:
                            S.op("dve", lambda e, k=k: e.reciprocal(out=stt2[:, k, 10:11], in_=stt2[:, k, 12:13]))
                        S.step()
                        for k in K2:
                            S.op("dve", lambda e, k=k: e.tensor_scalar(
                                out=o12[:, k, :], in0=o12[:, k, :], scalar1=stt2[:, k, 8:9], scalar2=stt2[:, k, 10:11],
                                op0=ALU.subtract, op1=ALU.mult))
                        S.step()
                        for k in K2:
                            S.op("dve" if k == 0 else "pool", lambda e, k=k: e.tensor_tensor(
                                out=retb2[:, k, :], in0=o12[:, k, :], in1=sgt2[:, k, :], op=ALU.mult))
                        S.step()
                        for k in K2:
                            for i in range(4):
                                S.op("pe", lambda e, i=i, k=k: e.transpose(
                                    out=ps_t2[k][:, i * 128:(i + 1) * 128], in_=retb2[:, k, i * 128:(i + 1) * 128],
                                    identity=identb[:]))
                        S.step()
                        for k in K2:
                            if k == 0:
                                S.op("act", lambda e, k=k: e.activation(
                                    out=rT2[:, k].rearrange("p a b -> p (a b)"), in_=ps_t2[k][:, :], func=AF.Copy))
                            else:
                                S.op("dve", lambda e, k=k: e.tensor_copy(
                                    out=rT2[:, k].rearrange("p a b -> p (a b)"), in_=ps_t2[k][:, :]))
                        S.step()
                        for k in K2:
                            S.dma("sp", RETT[:, h * 4:(h + 1) * 4, css[k]], rT2[:, k])
                        S.step()

        def stage_sgu(l):
            with ExitStack() as st:
                w32 = sb(st, "g_w32", [128, 8, 128], F32)
                w16 = sb(st, "g_w16", [128, 8, 128], BF16)
                wsT = sb(st, "g_wsT", [128, 8, 128], BF16)
                bsT = sb(st, "g_bsT", [128, 8], F32)
                lg_ = sb(st, "g_lg", [128, D], F32)
                lb_ = sb(st, "g_lb", [128, D], F32)
                va = sb(st, "g_va", [128, D], F32)
                uu = sb(st, "g_uu", [128, D], BF16)
                vn = sb(st, "g_vn", [128, D], BF16)
                s32 = sb(st, "g_s32", [128, D], F32)
                sT = sb(st, "g_sT", [128, 16, 128], BF16)
                stt = sb(st, "g_stt", [128, 32], F32)
                ps = [pb(st, "g_ps%d" % i, [128, 512], F32) for i in range(4)]
                pT = pb(st, "g_pT", [128, D], BF16)
                S.dma("sp", w32[:], sgu_w[l].rearrange("g p q -> p g q"))
                S.dma("sp", bsT[:], sgu_b[l].rearrange("g p -> p g"), allow_slow_non_contiguous=True)
                S.dma("sp", lg_[:], sgu_ln_g[l:l + 1, :].partition_broadcast(128))
                S.dma("sp", lb_[:], sgu_ln_b[l:l + 1, :].partition_broadcast(128))
                S.step()
                S.op("act", lambda e: e.activation(out=w16[:], in_=w32[:], func=AF.Copy))
                S.step()
                for g in range(8):
                    S.op("pe", lambda e, g=g: e.transpose(out=pT[:, g * 128:(g + 1) * 128], in_=w16[:, g, :],
                                                         identity=identb[:]))
                S.step()
                S.op("act", lambda e: e.activation(out=wsT[:].rearrange("p a b -> p (a b)"), in_=pT[:, 0:1024],
                                                   func=AF.Copy))
                S.step()
                for t in range(NT):
                    S.dma("sp", va[:], VA[t * 128:(t + 1) * 128, :])
                    S.dma("sp", uu[:], UU[t * 128:(t + 1) * 128, :])
                    S.step()
                    for i in range(4):
                        S.op("dve", lambda e, i=i: e.bn_stats(out=stt[:, i * 6:(i + 1) * 6],
                                                             in_=va[:, i * 512:(i + 1) * 512]))
                    S.step()
                    S.op("dve", lambda e: e.bn_aggr(out=stt[:, 24:26], in_=stt[:, 0:24].rearrange("p (a b) -> p a b", b=6)))
                    S.step()
                    S.op("dve", lambda e: e.tensor_scalar(out=stt[:, 27:28], in0=stt[:, 25:26], scalar1=EPS,
                                                         scalar2=None, op0=ALU.add))
                    S.step()
                    S.op("act", lambda e: e.activation(out=stt[:, 28:29], in_=stt[:, 27:28], func=AF.Sqrt))
                    S.step()
                    S.op("dve", lambda e: e.reciprocal(out=stt[:, 26:27], in_=stt[:, 28:29]))
                    S.step()
                    S.op("dve", lambda e: e.tensor_scalar(out=va[:], in0=va[:], scalar1=stt[:, 24:25],
                                                         scalar2=stt[:, 26:27], op0=ALU.subtract, op1=ALU.mult))
                    S.step()
                    S.op("dve", lambda e: e.tensor_tensor(out=va[:], in0=va[:], in1=lg_[:], op=ALU.mult))
                    S.step()
                    S.op("dve", lambda e: e.tensor_tensor(out=vn[:], in0=va[:], in1=lb_[:], op=ALU.add))
                    S.step()
                    for g in range(8):
                        S.op("pe", lambda e, g=g: e.matmul(ps[g // 2][:, (g % 2) * 256:(g % 2 + 1) * 256],
                                                          lhsT=wsT[:, g, :], rhs=vn[:, g * 256:(g + 1) * 256],
                                                          start=True, stop=True))
                    S.step()
                    for g in range(8):
                        S.op("dve", lambda e, g=g: e.tensor_scalar(
                            out=s32[:, g * 256:(g + 1) * 256], in0=ps[g // 2][:, (g % 2) * 256:(g % 2 + 1) * 256],
                            scalar1=bsT[:, g:g + 1], scalar2=None, op0=ALU.add))
                    S.step()
                    S.op("dve", lambda e: e.tensor_tensor(out=vn[:], in0=s32[:], in1=uu[:], op=ALU.mult))
                    S.step()
                    for kc in range(16):
                        S.op("pe", lambda e, kc=kc: e.transpose(out=pT[:, kc * 128:(kc + 1) * 128],
                                                               in_=vn[:, kc * 128:(kc + 1) * 128], identity=identb[:]))
                    S.step()
                    S.op("act", lambda e: e.activation(out=sT[:].rearrange("p a b -> p (a b)"), in_=pT[:, :], func=AF.Copy))
                    S.step()
                    S.dma("sp", SGT[:, :, t * 128:(t + 1) * 128], sT[:])
                    S.step()

        def fm_epilogue(st, tag, func, mul_src, add_src, dst, dst_dt, cb_off=0, grp=None):
            G_ = grp or groups
            m16 = sb(st, tag + "_m", [128, 2, 4, 512], BF16) if mul_src is not None else None
            a32 = sb(st, tag + "_a", [128, 2, 4, 512], F32) if add_src is not None else None
            o = sb(st, tag + "_o", [128, 2, 4, 512], dst_dt)
            t32 = sb(st, tag + "_t", [128, 4, 512], F32)

            def pre(nb, gi, slot):
                tok0, ntok = G_[gi]
                if mul_src is not None:
                    S.dma_async("sp", m16[:, slot, :, 0:ntok],
                                mul_src[:, cb_off + nb * 4:cb_off + nb * 4 + 4, tok0:tok0 + ntok], tok_pre[slot])
                if add_src is not None:
                    S.dma_async("sp", a32[:, slot, :, 0:ntok],
                                add_src[:, cb_off + nb * 4:cb_off + nb * 4 + 4, tok0:tok0 + ntok], tok_pre[slot])

            def epi(ps, nb, gi, slot):
                tok0, ntok = G_[gi]
                if mul_src is None:
                    for cb in range(4):
                        S.op("act", lambda e, cb=cb: e.activation(out=o[:, slot, cb, 0:ntok], in_=ps[cb][:, 0:ntok], func=func))
                    S.step()
                else:
                    tgt = o[:, slot] if add_src is None else t32
                    for cb in range(4):
                        eng = "dve" if cb % 2 == 0 else "pool"
                        if func == AF.Copy:
                            S.op("dve", lambda e, cb=cb, tgt=tgt: e.tensor_tensor(
                                out=tgt[:, cb, 0:ntok], in0=ps[cb][:, 0:ntok], in1=m16[:, slot, cb, 0:ntok], op=ALU.mult))
                        else:
                            S.op("act", lambda e, cb=cb: e.activation(out=t32[:, cb, 0:ntok], in_=ps[cb][:, 0:ntok], func=func))
                    S.step()
                    if func != AF.Copy:
                        S.op("dve", lambda e, tgt=tgt: e.tensor_tensor(out=tgt[:, :, 0:ntok], in0=t32[:, :, 0:ntok],
                                                                      in1=m16[:, slot, :, 0:ntok], op=ALU.mult))
                        S.step()
                    if add_src is not None:
                        S.op("pool", lambda e: e.tensor_tensor(out=o[:, slot, :, 0:ntok], in0=t32[:, :, 0:ntok],
                                                              in1=a32[:, slot, :, 0:ntok], op=ALU.add))
                        S.step()
                S.dma_async("sp", dst[:, cb_off + nb * 4:cb_off + nb * 4 + 4, tok0:tok0 + ntok], o[:, slot, :, 0:ntok], tok_st)
                S.step()
            return (pre if (mul_src is not None or add_src is not None) else None), epi

        def resid_epilogue(st, tag, mb_idx, gate_e=None, grp=None):
            G_ = grp or groups
            cv = sb(st, tag + "_cv", [128, 2, D], F32)
            xb = sb(st, tag + "_xb", [128, 2, 4, 512], F32)
            tm = sb(st, tag + "_tm", [128, 4, 512], F32)
            gt = sb(st, tag + "_gt", [128, 2, 4, NE], F32)
            for j in range(2):
                S.dma("sp", cv[:, j, :], MB[j, :, mb_idx * D:(mb_idx + 1) * D])
            S.step()

            def pre(nb, gi, slot):
                tok0, ntok = G_[gi]
                nj = ntok // 128
                S.dma_async("sp", xb[:, slot, 0:nj, :],
                            XR[tok0:tok0 + ntok, nb * 512:(nb + 1) * 512].rearrange("(j p) n -> p j n", p=128),
                            tok_pre[slot])
                if gate_e is not None:
                    S.dma_async("sp", gt[:, slot, 0:nj, :],
                                GT[tok0:tok0 + ntok, :].rearrange("(j p) n -> p j n", p=128), tok_pre[slot])

            def epi(ps, nb, gi, slot):
                tok0, ntok = G_[gi]
                nj = ntok // 128
                jc = 1 if tok0 < NCTX else 0
                for j in range(nj):
                    if gate_e is None:
                        S.op("dve", lambda e, j=j: e.tensor_tensor(out=tm[:, j, :], in0=ps[j][:, :],
                                                                  in1=cv[:, jc, nb * 512:(nb + 1) * 512], op=ALU.mult))
                    else:
                        S.op("dve", lambda e, j=j: e.scalar_tensor_tensor(
                            out=tm[:, j, :], in0=ps[j][:, :], scalar=gt[:, slot, j, gate_e:gate_e + 1],
                            in1=cv[:, jc, nb * 512:(nb + 1) * 512], op0=ALU.mult, op1=ALU.mult))
                S.step()
                S.op("pool", lambda e: e.tensor_tensor(out=xb[:, slot, 0:nj, :], in0=xb[:, slot, 0:nj, :],
                                                      in1=tm[:, 0:nj, :], op=ALU.add))
                S.step()
                S.dma("sp", XR[tok0:tok0 + ntok, nb * 512:(nb + 1) * 512].rearrange("(j p) n -> p j n", p=128),
                      xb[:, slot, 0:nj, :])
                S.step()
            return pre, epi

        def stage_merge(l):
            with ExitStack() as st:
                pre, epi = fm_epilogue(st, "pa", AF.Copy, SGA, None, YT, F32)
                gemm(SGT, 16, w_proj_a[l], D, "fm", pre, epi, "pa")
            with ExitStack() as st:
                pre, epi = fm_epilogue(st, "pr", AF.Copy, SGR, YT, YTB, BF16)
                gemm(RETT, 32, w_proj_r[l], D, "fm", pre, epi, "pr")
            with ExitStack() as st:
                pre, epi = resid_epilogue(st, "wo", 2)
                gemm(YTB, 16, w_out[l], D, "tm", pre, epi, "wo")

        def stage_ffn_dense(l, di):
            with ExitStack() as st:
                pre, epi = fm_epilogue(st, "f1", AF.Silu, None, None, H1T, BF16)
                gemm(HT, 16, ffn_w1[di], D_FF, "fm", pre, epi, "f1")
            with ExitStack() as st:
                pre, epi = fm_epilogue(st, "f3", AF.Copy, H1T, None, HIDT, BF16)
                gemm(HT, 16, ffn_w3[di], D_FF, "fm", pre, epi, "f3")
            with ExitStack() as st:
                g256 = [(i * 256, 256) for i in range(T // 256)]
                pre, epi = resid_epilogue(st, "f2", 5, grp=g256)
                gemm(HIDT, 44, ffn_w2[di], D, "tm", pre, epi, "f2", grp=g256)

        def stage_ffn_moe(l, mi):
            for e_ in range(NE):
                with ExitStack() as st:
                    pre, epi = fm_epilogue(st, "m1", AF.Silu, None, None, H1T, BF16)
                    gemm(HT, 16, moe_w1[mi, e_], D_FFE, "fm", pre, epi, "m1")
                with ExitStack() as st:
                    pre, epi = fm_epilogue(st, "m3", AF.Copy, H1T, None, HIDT, BF16)
                    gemm(HT, 16, moe_w3[mi, e_], D_FFE, "fm", pre, epi, "m3")
                with ExitStack() as st:
                    pre, epi = resid_epilogue(st, "m2", 5, gate_e=e_)
                    gemm(HIDT, 32, moe_w2[mi, e_], D, "tm", pre, epi, "m2")

        def stage_final():
            with ExitStack() as st:
                gB = sb(st, "fgB", [128, D], F32)
                x = sb(st, "fx", [128, D], F32)
                junk = sb(st, "fjunk", [128, D], F32)
                ss = sb(st, "fss", [128, 4], F32)
                if final_norm:
                    S.dma("sp", gB[:], final_g[0:1, :].partition_broadcast(128))
                    S.step()
                S.dma("sp", x_out.rearrange("(p a) d -> p (a d)", p=128), XR.rearrange("(p a) d -> p (a d)", p=128))
                S.step()
                for t in range(NCTX // 128, NT):
                    S.dma("sp", x[:], XR[t * 128:(t + 1) * 128, :])
                    S.op("pool", lambda e: e.memset(ss[:], 0.0))
                    S.step()
                    if final_norm:
                        S.op("act", lambda e: e.activation(out=junk[:], in_=x[:], func=AF.Square, accum_out=ss[:, 0:1]))
                        S.step()
                        S.op("dve", lambda e: e.tensor_scalar(out=ss[:, 1:2], in0=ss[:, 0:1], scalar1=1.0 / D,
                                                             scalar2=EPS, op0=ALU.mult, op1=ALU.add))
                        S.step()
                        S.op("act", lambda e: e.activation(out=ss[:, 3:4], in_=ss[:, 1:2], func=AF.Sqrt))
                        S.step()
                        S.op("dve", lambda e: e.reciprocal(out=ss[:, 2:3], in_=ss[:, 3:4]))
                        S.step()
                        S.op("dve", lambda e: e.scalar_tensor_tensor(out=x[:], in0=x[:], scalar=ss[:, 2:3], in1=gB[:],
                                                                    op0=ALU.mult, op1=ALU.mult))
                        S.step()
                    S.dma("sp", y_out[t * 128 - NCTX:(t + 1) * 128 - NCTX, :], x[:])
                    S.step()

        di = mi = 0
        for l, kind in enumerate(layer_kinds):
            stage_mod(l)
            stage_norm(l, 1)
            stage_inproj(l)
            with ExitStack() as dst_:
                dec = stage_decays(l, dst_)
                stage_rope(l, dec)
                stage_retention(l, dec)
            stage_sgu(l)
            stage_merge(l)
            if kind == "dense":
                stage_norm(l, 2)
                stage_ffn_dense(l, di)
                di += 1
            else:
                stage_norm(l, 2, router=mi)
                stage_ffn_moe(l, mi)
                mi += 1
        stage_final()
        for sm, v in S.prev:
            nc.sync.wait_ge(sm, v)
    return nc


def make_consts():
    i = np.arange(128, dtype=np.float32)
    d = i[None, :] - i[:, None]
    c = np.zeros((128, 5 * 128 + 8), np.float32)
    c[:, 0:128] = np.eye(128, dtype=np.float32)
    c[:, 128:256] = np.maximum(d, 0)
    c[:, 256:384] = np.maximum(-d, 0)
    c[:, 384:512] = (d >= 0)
    c[:, 512:640] = (d <= 0)
    c[:, 640] = i
    c[:, 641] = 127 - i
    c[:, 642] = i + 1
    c[:, 643] = 128 - i
    return c


def make_rope(n_lat):
    half = 64
    freqs = (10000.0 ** (-np.arange(half, dtype=np.float32) / half)).astype(np.float32)
    t = np.arange(n_lat)
    rows = (t // GRID_W).astype(np.float32)
    cols = (t % GRID_W).astype(np.float32)
    ang_r = rows[:, None] * freqs[None, :]
    ang_c = cols[:, None] * freqs[None, :]
    out = np.zeros((n_lat, 256), np.float32)
    out[:, 0:64] = np.cos(ang_r)
    out[:, 64:128] = np.cos(ang_c)
    out[:, 128:192] = np.sin(ang_r)
    out[:, 192:256] = np.sin(ang_c)
    return out.reshape(n_lat // 128, 128, 256)


def run(inputs, layer_kinds, final_norm=True, cores=None):
    x = np.asarray(inputs["x"], np.float32)
    B, n_lat, _ = x.shape
    L = len(layer_kinds)
    nd = max(1, sum(1 for k in layer_kinds if k == "dense"))
    nm = max(1, L - sum(1 for k in layer_kinds if k == "dense"))
    nc = build(n_lat, layer_kinds, final_norm)
    f = lambda k: np.asarray(inputs[k], np.float32)
    sl = lambda k, n: np.ascontiguousarray(f(k)[:n])
    shared = {
        "w_mod": sl("w_mod", L), "b_mod": sl("b_mod", L), "norm1_g": sl("norm1_g", L), "norm2_g": sl("norm2_g", L),
        "w_in": sl("w_in", L), "sgu_ln_g": sl("sgu_ln_g", L), "sgu_ln_b": sl("sgu_ln_b", L),
        "sgu_w": sl("sgu_w", L), "sgu_b": sl("sgu_b", L),
        "ret_log_decay": sl("ret_log_decay", L).reshape(L, 16),
        "w_proj_a": sl("w_proj_a", L), "w_proj_r": sl("w_proj_r", L), "w_out": sl("w_out", L),
        "ffn_w1": sl("ffn_w1", nd), "ffn_w3": sl("ffn_w3", nd), "ffn_w2": sl("ffn_w2", nd),
        "router_w": sl("router_w", nm), "router_b": sl("router_b", nm),
        "moe_w1": sl("moe_w1", nm), "moe_w3": sl("moe_w3", nm), "moe_w2": sl("moe_w2", nm),
        "final_norm_g": f("final_norm_g").reshape(1, D), "consts": make_consts(), "rope_t": make_rope(n_lat),
    }
    in_maps = []
    for b in range(B):
        m = dict(shared)
        m["xin"] = np.concatenate([f("ctx")[b], x[b]], axis=0)
        m["cvec"] = np.stack([f("c")[b], f("c_ctx")], axis=0)
        in_maps.append(m)
    res = run_bass_kernel_spmd(nc, in_maps, core_ids=list(range(B)))
    return np.stack([np.asarray(res.results[b]["y_out"], np.float32) for b in range(B)], axis=0)


def kernel(**inputs):
    return run(inputs, ["dense", "moe", "dense", "moe"], True)
```
